# Optimizing a Trainium2 kernel written in Bass

```python
import jax, jax.numpy as jnp
from jax import lax
import numpy as np

D_MODEL = 1024
BATCH = 16
SEQ = 2048
DEPTH = 1

GRID_W = 64
CTX_LEN = 256
EPS = 1e-6
N_FOURIER_GROUPS = 4
FOURIER_GROUP_DIM = 128
D_FOURIER = N_FOURIER_GROUPS * FOURIER_GROUP_DIM
N_LRU_HEADS = 8
LRU_HEAD_DIM = 64
D_LRU = N_LRU_HEADS * LRU_HEAD_DIM
CONV_WIDTH = 4
CONV_LEFT = 2
LRU_C = 8.0
D_IN = D_FOURIER + 2 * D_LRU + 2 * D_MODEL
N_GROUPS = 4
EXPERTS_PER_GROUP = 8
N_EXPERTS = N_GROUPS * EXPERTS_PER_GROUP
TOP_K = 2
D_EXPERT = 512
MOE_BLOCK = 128

kernel_name = "hybrid_fnet_rglru_hmoe_dit"


def rmsnorm(x, g):
    xf = x.astype(jnp.float32)
    y = xf * lax.rsqrt(jnp.mean(xf * xf, axis=-1, keepdims=True) + EPS)
    return (y * g.astype(jnp.float32)).astype(x.dtype)


def modulate(x, shift, scale):
    return x * (1 + scale) + shift


def fourier_mix(u, grid_w):
    b_, l_, _ = u.shape
    uf = u.astype(jnp.float32)
    if grid_w is not None:
        rows = l_ // grid_w
        uf = uf.reshape(b_, rows, grid_w, N_FOURIER_GROUPS, FOURIER_GROUP_DIM)
        out = jnp.real(jnp.fft.fftn(uf, axes=(1, 2, 4), norm="ortho"))
    else:
        uf = uf.reshape(b_, l_, N_FOURIER_GROUPS, FOURIER_GROUP_DIM)
        out = jnp.real(jnp.fft.fftn(uf, axes=(1, 3), norm="ortho"))
    return out.reshape(b_, l_, D_FOURIER).astype(u.dtype)


def short_conv(u, w, b):
    l_ = u.shape[1]
    up = jnp.pad(u, ((0, 0), (CONV_LEFT, CONV_WIDTH - 1 - CONV_LEFT), (0, 0)))
    return sum(up[:, k:k + l_] * w[k] for k in range(CONV_WIDTH)) + b


def lru_coeffs(xc, w_a, b_a, w_x, b_x, lam):
    b_, l_, _ = xc.shape
    xh = xc.reshape(b_, l_, N_LRU_HEADS, LRU_HEAD_DIM)
    r = jax.nn.sigmoid(jnp.einsum('blhi,hij->blhj', xh, w_a).reshape(b_, l_, D_LRU) + b_a)
    i = jax.nn.sigmoid(jnp.einsum('blhi,hij->blhj', xh, w_x).reshape(b_, l_, D_LRU) + b_x)
    log_a = -LRU_C * r.astype(jnp.float32) * jax.nn.softplus(-lam.astype(jnp.float32))
    a = jnp.exp(log_a)
    mult = jnp.sqrt(-jnp.expm1(2.0 * log_a))
    return a, mult * (i * xc).astype(jnp.float32)


def linear_scan(a, b, h0, reverse):
    if reverse:
        a, b = jnp.flip(a, 1), jnp.flip(b, 1)
    b = b.at[:, 0].add(a[:, 0] * h0)

    def combine(l, r):
        return (l[0] * r[0], r[0] * l[1] + r[1])

    _, h = lax.associative_scan(combine, (a, b), axis=1)
    return jnp.flip(h, 1) if reverse else h


def bidir_rglru(u, conv_w, conv_b, w_a, b_a, w_x, b_x, lam, h0):
    xc = short_conv(u, conv_w, conv_b)
    a_f, b_f = lru_coeffs(xc, w_a[0], b_a[0], w_x[0], b_x[0], lam[0])
    a_b, b_b = lru_coeffs(xc, w_a[1], b_a[1], w_x[1], b_x[1], lam[1])
    return linear_scan(a_f, b_f, h0[0], False), linear_scan(a_b, b_b, h0[1], True)


def token_mixer(h, w_in, conv_w, conv_b, w_a, b_a, w_x, b_x, lam, w_fo, w_lo, w_out, h0, grid_w):
    proj = h @ w_in
    u_f = proj[..., :D_FOURIER]
    u_r = proj[..., D_FOURIER:D_FOURIER + D_LRU]
    y_r = proj[..., D_FOURIER + D_LRU:D_FOURIER + 2 * D_LRU]
    gate_f, gate_r = jnp.split(jax.nn.sigmoid(proj[..., D_FOURIER + 2 * D_LRU:]), 2, axis=-1)
    branch_f = fourier_mix(u_f, grid_w) @ w_fo
    h_f, h_b = bidir_rglru(u_r, conv_w, conv_b, w_a, b_a, w_x, b_x, lam, h0)
    branch_r = ((h_f + h_b) * jax.nn.gelu(y_r.astype(jnp.float32))).astype(h.dtype) @ w_lo
    return (gate_f * branch_f + gate_r * branch_r) @ w_out, h_f, h_b


def hier_moe(h, w_group, b_group, w_er, b_er, w_g, w_u, w_d):
    b_, l_, d_ = h.shape
    t = h.reshape(-1, d_)
    n = t.shape[0]
    tf = t.astype(jnp.float32)
    g_logits = tf @ w_group.astype(jnp.float32) + b_group.astype(jnp.float32)
    g_prob = jax.nn.softmax(g_logits, axis=-1)
    grp = jnp.argmax(g_logits, axis=-1)
    p_grp = jnp.take_along_axis(g_prob, grp[:, None], axis=1)[:, 0]
    e_logits = (tf @ w_er.astype(jnp.float32) + b_er.astype(jnp.float32)).reshape(n, N_GROUPS, EXPERTS_PER_GROUP)
    e_sel = jnp.take_along_axis(e_logits, grp[:, None, None], axis=1)[:, 0]
    top_l, top_i = lax.top_k(e_sel, TOP_K)
    w = p_grp[:, None] * jax.nn.softmax(top_l, axis=-1)
    eid = grp[:, None] * EXPERTS_PER_GROUP + top_i

    flat_e = eid.reshape(-1)
    flat_w = w.reshape(-1)
    n_assign = n * TOP_K
    order = jnp.argsort(flat_e)
    se = flat_e[order]
    stok = (order // TOP_K).astype(jnp.int32)
    sw = flat_w[order]
    counts = jnp.bincount(flat_e, length=N_EXPERTS)
    padded = (counts + MOE_BLOCK - 1) // MOE_BLOCK * MOE_BLOCK
    start = jnp.cumsum(counts) - counts
    pend = jnp.cumsum(padded)
    pstart = pend - padded
    pos = pstart[se] + jnp.arange(n_assign) - start[se]
    n_rows = n_assign + N_EXPERTS * MOE_BLOCK
    n_blk = n_rows // MOE_BLOCK
    row_tok = jnp.full((n_rows,), n, jnp.int32).at[pos].set(stok)
    row_w = jnp.zeros((n_rows,), jnp.float32).at[pos].set(sw)
    blk_e = jnp.minimum(jnp.searchsorted(pend, jnp.arange(n_blk) * MOE_BLOCK, side='right'), N_EXPERTS - 1)
    t_pad = jnp.concatenate([t, jnp.zeros((1, d_), t.dtype)], axis=0)
    xs = t_pad[row_tok].reshape(n_blk, MOE_BLOCK, d_)

    def expert_block(args):
        xb, e = args
        hb = jax.nn.silu(xb @ w_g[e]) * (xb @ w_u[e])
        return hb @ w_d[e]

    ys = lax.map(expert_block, (xs, blk_e)).reshape(n_rows, d_)
    out = jnp.zeros((n + 1, d_), jnp.float32).at[row_tok].add(ys.astype(jnp.float32) * row_w[:, None])[:n]
    return out.reshape(b_, l_, d_).astype(h.dtype)


def setup_inputs(seed: int = 0) -> dict:
    key = jax.random.key(seed)
    ks = jax.random.split(key, 28)

    def nrm(k, shape, scale):
        return jax.random.normal(k, shape, jnp.float32) * scale

    s_d = D_MODEL ** -0.5
    a0 = jax.random.uniform(ks[14], (DEPTH, 2, D_LRU), jnp.float32, 0.9, 0.999)
    s = a0 ** (1.0 / LRU_C)
    lam = jnp.log(s) - jnp.log1p(-s)
    return {
        "x": nrm(ks[0], (BATCH, SEQ, D_MODEL), 1.0),
        "c": nrm(ks[1], (BATCH, D_MODEL), 1.0),
        "ctx": nrm(ks[2], (BATCH, CTX_LEN, D_MODEL), 1.0),
        "c_ctx": nrm(ks[3], (D_MODEL,), 1.0),
        "w_mod": nrm(ks[4], (DEPTH, D_MODEL, 6 * D_MODEL), 0.5 * s_d),
        "b_mod": nrm(ks[5], (DEPTH, 6 * D_MODEL), 0.01),
        "norm1_g": 1.0 + nrm(ks[6], (DEPTH, D_MODEL), 0.01),
        "w_in": nrm(ks[7], (DEPTH, D_MODEL, D_IN), s_d),
        "conv_w": nrm(ks[8], (DEPTH, CONV_WIDTH, D_LRU), CONV_WIDTH ** -0.5),
        "conv_b": nrm(ks[9], (DEPTH, D_LRU), 0.01),
        "lru_wa": nrm(ks[10], (DEPTH, 2, N_LRU_HEADS, LRU_HEAD_DIM, LRU_HEAD_DIM), LRU_HEAD_DIM ** -0.5),
        "lru_ba": nrm(ks[11], (DEPTH, 2, D_LRU), 0.01),
        "lru_wx": nrm(ks[12], (DEPTH, 2, N_LRU_HEADS, LRU_HEAD_DIM, LRU_HEAD_DIM), LRU_HEAD_DIM ** -0.5),
        "lru_bx": nrm(ks[13], (DEPTH, 2, D_LRU), 0.01),
        "lru_lam": lam,
        "w_fourier_out": nrm(ks[15], (DEPTH, D_FOURIER, D_MODEL), D_FOURIER ** -0.5),
        "w_lru_out": nrm(ks[16], (DEPTH, D_LRU, D_MODEL), D_LRU ** -0.5),
        "w_out": nrm(ks[17], (DEPTH, D_MODEL, D_MODEL), s_d),
        "norm2_g": 1.0 + nrm(ks[18], (DEPTH, D_MODEL), 0.01),
        "w_group": nrm(ks[19], (DEPTH, D_MODEL, N_GROUPS), s_d),
        "b_group": nrm(ks[20], (DEPTH, N_GROUPS), 0.01),
        "w_expert_router": nrm(ks[21], (DEPTH, D_MODEL, N_EXPERTS), s_d),
        "b_expert_router": nrm(ks[22], (DEPTH, N_EXPERTS), 0.01),
        "w_gate_e": nrm(ks[23], (DEPTH, N_EXPERTS, D_MODEL, D_EXPERT), s_d),
        "w_up_e": nrm(ks[24], (DEPTH, N_EXPERTS, D_MODEL, D_EXPERT), s_d),
        "w_down_e": nrm(ks[25], (DEPTH, N_EXPERTS, D_EXPERT, D_MODEL), D_EXPERT ** -0.5),
        "final_g": 1.0 + nrm(ks[26], (D_MODEL,), 0.01),
    }


def reference(x, c, ctx, c_ctx, w_mod, b_mod, norm1_g, w_in, conv_w, conv_b, lru_wa, lru_ba, lru_wx, lru_bx,
              lru_lam, w_fourier_out, w_lru_out, w_out, norm2_g, w_group, b_group, w_expert_router,
              b_expert_router, w_gate_e, w_up_e, w_down_e, final_g):
    for l in range(DEPTH):
        mod = (jax.nn.silu(c) @ w_mod[l] + b_mod[l])[:, None, :]
        sh1, sc1, g1, sh2, sc2, g2 = jnp.split(mod, 6, axis=-1)
        modc = jax.nn.silu(c_ctx) @ w_mod[l] + b_mod[l]
        csh1, csc1, cg1, csh2, csc2, cg2 = jnp.split(modc, 6, axis=-1)
        lru_p = (conv_w[l], conv_b[l], lru_wa[l], lru_ba[l], lru_wx[l], lru_bx[l], lru_lam[l])
        zeros_h = jnp.zeros((2, ctx.shape[0], D_LRU), jnp.float32)

        hc = modulate(rmsnorm(ctx, norm1_g[l]), csh1, csc1)
        if l < DEPTH - 1:
            mix_c, hcf, hcb = token_mixer(hc, w_in[l], *lru_p, w_fourier_out[l], w_lru_out[l], w_out[l],
                                          zeros_h, None)
        else:
            uc = hc @ w_in[l][:, D_FOURIER:D_FOURIER + D_LRU]
            hcf, hcb = bidir_rglru(uc, *lru_p, zeros_h)
        h0 = jnp.stack([hcf[:, -1], hcb[:, 0]])

        hx = modulate(rmsnorm(x, norm1_g[l]), sh1, sc1)
        mix_x, _, _ = token_mixer(hx, w_in[l], *lru_p, w_fourier_out[l], w_lru_out[l], w_out[l], h0, GRID_W)
        x = x + g1 * mix_x

        moe_p = (w_group[l], b_group[l], w_expert_router[l], b_expert_router[l], w_gate_e[l], w_up_e[l],
                 w_down_e[l])
        x = x + g2 * hier_moe(modulate(rmsnorm(x, norm2_g[l]), sh2, sc2), *moe_p)

        if l < DEPTH - 1:
            ctx = ctx + cg1 * mix_c
            ctx = ctx + cg2 * hier_moe(modulate(rmsnorm(ctx, norm2_g[l]), csh2, csc2), *moe_p)
    return rmsnorm(x, final_g)
```

```python
import contextlib
import numpy as np
import ml_dtypes
import concourse.bass as bass
import concourse.mybir as mybir
from concourse.bass_utils import run_bass_kernel_spmd

F32 = mybir.dt.float32
BF16 = mybir.dt.bfloat16
U32 = mybir.dt.uint32
I32 = mybir.dt.int32
AF = mybir.ActivationFunctionType
ALU = mybir.AluOpType
AX = mybir.AxisListType

ENGS = ("tensor", "vector", "scalar", "gpsimd", "sync")


class Prog:
    def __init__(self, nc, stack):
        self.nc = nc
        self.stack = stack
        self.q = {e: [] for e in ENGS}
        self.esem = {e: stack.enter_context(nc.semaphore("prog_" + e)) for e in ENGS}
        self.ecnt = {e: 0 for e in ENGS}
        self.waited = {e: {} for e in ENGS}
        self.dsem = {}
        self.dcnt = {}
        self.last_w = {}
        self.reads = {}
        self.sems = {}
        for e in ENGS:
            self.sems[id(self.esem[e])] = self.esem[e]

    def _dma_sem(self, key):
        if key not in self.dsem:
            s = self.stack.enter_context(self.nc.semaphore("dma_%d" % len(self.dsem)))
            self.dsem[key] = s
            self.dcnt[key] = 0
            self.sems[id(s)] = s
        return self.dsem[key]

    def _deps(self, eng, reads, writes):
        evs = []
        for r in reads:
            if r in self.last_w:
                evs.append(self.last_w[r])
        for w in writes:
            if w in self.last_w:
                evs.append(self.last_w[w])
            evs.extend(self.reads.get(w, ()))
        need = {}
        for (sid, val) in evs:
            if sid == id(self.esem[eng]) and eng == "tensor":
                continue
            if self.waited[eng].get(sid, 0) >= val:
                continue
            if need.get(sid, 0) < val:
                need[sid] = val
        for sid, val in need.items():
            self.waited[eng][sid] = val
            sem = self.sems[sid]
            self.q[eng].append(lambda e, sem=sem, val=val: e.wait_ge(sem, val))

    def _commit(self, ev, reads, writes):
        for w in writes:
            self.last_w[w] = ev
            self.reads[w] = []
        for r in reads:
            if r in writes:
                continue
            self.reads.setdefault(r, []).append(ev)

    def op(self, eng, fn, reads=(), writes=()):
        reads, writes = tuple(reads), tuple(writes)
        self._deps(eng, reads, writes)
        self.ecnt[eng] += 1
        sem = self.esem[eng]
        self.q[eng].append(lambda e, fn=fn, sem=sem: fn(e).then_inc(sem, 1))
        self._commit((id(sem), self.ecnt[eng]), reads, writes)

    def dma(self, eng, fn, key, reads=(), writes=(), serial=False):
        reads, writes = tuple(reads), tuple(writes)
        sem = self._dma_sem(key)
        prev = self.dcnt[key]
        if serial and prev and self.waited[eng].get(id(sem), 0) < prev:
            self.waited[eng][id(sem)] = prev
            self.q[eng].append(lambda e, sem=sem, val=prev: e.wait_ge(sem, val))
        self._deps(eng, reads, writes)
        self.dcnt[key] += 16
        self.q[eng].append(lambda e, fn=fn, sem=sem: fn(e).then_inc(sem, 16))
        self._commit((id(sem), self.dcnt[key]), reads, writes)

    def finish(self, out_keys):
        self._deps("sync", out_keys, ())
        nc = self.nc
        with nc.Block() as block:
            @block.tensor
            def _(e):
                for f in self.q["tensor"]:
                    f(e)

            @block.vector
            def _(e):
                for f in self.q["vector"]:
                    f(e)

            @block.scalar
            def _(e):
                for f in self.q["scalar"]:
                    f(e)

            @block.gpsimd
            def _(e):
                for f in self.q["gpsimd"]:
                    f(e)

            @block.sync
            def _(e):
                for f in self.q["sync"]:
                    f(e)

    def barrier(self, exclude=()):
        evs = [(id(self.esem[e]), self.ecnt[e]) for e in ENGS if self.ecnt[e]]
        evs += [(id(self.dsem[k]), self.dcnt[k]) for k in self.dsem if self.dcnt[k] and k not in exclude]
        for eng in ENGS:
            for sid, val in evs:
                if sid == id(self.esem[eng]):
                    continue
                if self.waited[eng].get(sid, 0) >= val:
                    continue
                self.waited[eng][sid] = val
                sem = self.sems[sid]
                self.q[eng].append(lambda e, sem=sem, val=val: e.wait_ge(sem, val))


D = 1024
NB = 2
T = 2048
TC = 256
NTOK = NB * T
DIN = 3584
NE = 32
DEXP = 512
CAP = 640
CT = CAP // 128
NSLOT = NE * CAP
EPS = 1e-6
KB = 1024


def _consts():
    c = {}
    ch = np.arange(128)
    th = 2 * np.pi * np.outer(ch, ch) / 128.0
    s = 1.0 / np.sqrt(128.0)
    c["cs"] = np.concatenate([np.cos(th) * s, np.sin(th) * s], axis=1).astype(ml_dtypes.bfloat16)
    p = np.arange(2048)
    r, cc = p // 64, p % 64
    ph = (np.outer(r, r) / 32.0 + np.outer(cc, cc) / 64.0)
    ph = 2 * np.pi * (ph - np.floor(ph))
    s2 = 1.0 / np.sqrt(2048.0)
    c["cp"] = (np.cos(ph) * s2).astype(ml_dtypes.bfloat16)
    c["spn"] = (-np.sin(ph) * s2).astype(ml_dtypes.bfloat16)
    c["idb"] = np.eye(128).astype(ml_dtypes.bfloat16)
    c["idf"] = np.eye(128).astype(np.float32)
    c["ltri"] = np.triu(np.ones((128, 128)), 1).astype(ml_dtypes.bfloat16)
    c["ones"] = np.ones((128, 128)).astype(ml_dtypes.bfloat16)
    c["ebase"] = np.tile((np.arange(NE) * CAP).astype(np.float32)[None, :], (128, 1))
    return c


def build_program(debug=()):
    nc = bass.Bass("TRN2", target_bir_lowering=False)
    dbg = set(debug)

    def din(name, shape, dt=F32):
        return nc.dram_tensor(name, list(shape), dt, kind="ExternalInput").ap()

    x_d = din("x", [NTOK, D])
    ctx_d = din("ctx", [NB * TC, D])
    cT_d = din("cT", [128, 8, 3])
    wmod_d = din("w_mod", [D, 6 * D])
    bmod_d = din("b_mod", [1, 6 * D])
    n1g_d = din("norm1_g", [1, D])
    n2g_d = din("norm2_g", [1, D])
    fg_d = din("final_g", [1, D])
    win_d = din("w_in", [D, DIN])
    wfo_d = din("w_fo", [512, D])
    wlo_d = din("w_lo", [512, D])
    wout_d = din("w_out", [D, D])
    lruc_d = din("lruc", [128, 4, 16])
    wbd_d = din("wbd", [128, 16, 128])
    wr_d = din("wr", [128, 8, 36])
    br_d = din("br", [1, 36])
    wg_d = din("w_gate_e", [NE * D, DEXP])
    wu_d = din("w_up_e", [NE * D, DEXP])
    wd_d = din("w_down_e", [NE * DEXP, D])
    cs_d = din("cs", [128, 256], BF16)
    cp_d = din("cp", [2048, 2048], BF16)
    spn_d = din("spn", [2048, 2048], BF16)
    idb_d = din("idb", [128, 128], BF16)
    idf_d = din("idf", [128, 128], F32)
    ltri_d = din("ltri", [128, 128], BF16)
    ones_d = din("ones", [128, 128], BF16)
    ebase_d = din("ebase", [128, NE], F32)

    out_d = nc.dram_tensor("out", [NTOK, D], F32, kind="ExternalOutput").ap()
    mod_d = nc.dram_tensor("mod_scr", [3, 6 * D], F32, kind="Internal").ap()
    x1_d = nc.dram_tensor("x1_scr", [NTOK, D], F32, kind="Internal").ap()
    xs_d = nc.dram_tensor("xs_scr", [NSLOT, D], BF16, kind="Internal").ap()
    ys_d = nc.dram_tensor("ys_scr", [NSLOT, D], F32, kind="Internal").ap()
    dbg_d = {}

    def dout(name, shape, dt=F32):
        dbg_d[name] = nc.dram_tensor("dbg_" + name, list(shape), dt, kind="ExternalOutput").ap()
        return dbg_d[name]

    with contextlib.ExitStack() as st:
        P = Prog(nc, st)

        def sb(name, shape, dt):
            return st.enter_context(nc.sbuf_tensor("s_" + name, list(shape), dt))

        arena = sb("arena", [128, 160 * KB // 4], F32)

        def region(off_kb, shape, dt):
            n = int(np.prod(shape[1:]))
            if dt == F32:
                v = arena[:, off_kb * 256: off_kb * 256 + n]
            else:
                v = arena[:, off_kb * 256: off_kb * 256 + n // 2].bitcast(BF16)
            if len(shape) == 3:
                v = v.rearrange("p (a b) -> p a b", a=shape[1])
            elif len(shape) == 4:
                v = v.rearrange("p (a b c) -> p a b c", a=shape[1], b=shape[2])
            elif len(shape) == 5:
                v = v.rearrange("p (a b c d) -> p a b c d", a=shape[1], b=shape[2], c=shape[3])
            return v

        psum = [st.enter_context(nc.psum_tensor("ps%d" % i, [128, 512], F32)) for i in range(8)]
        ps_i = [0]

        def next_ps():
            i = ps_i[0]
            ps_i[0] = (i + 1) % 8
            return psum[i], "ps%d" % i

        idb = sb("idb", [128, 128], BF16)
        idf = sb("idf", [128, 128], F32)
        cs = sb("cs", [128, 256], BF16)
        ltri = sb("ltri", [128, 128], BF16)
        ones = sb("ones", [128, 128], BF16)
        ebase = sb("ebase", [128, NE], F32)
        lruc = sb("lruc", [128, 4, 16], F32)
        lrud = sb("lrud", [128, 4, 8], F32)
        wbd = sb("wbd", [128, 16, 128], BF16)
        wr = sb("wr", [128, 8, 36], F32)
        brr = sb("brr", [128, 36], F32)
        onecol = sb("onecol", [128, 1], F32)
        for t_, d_, q in ((idb, idb_d, "sync"), (idf, idf_d, "sync"), (cs, cs_d, "sync"), (ltri, ltri_d, "sync"),
                          (ones, ones_d, "sync"), (ebase, ebase_d, "sync"), (lruc, lruc_d, "sync"),
                          (wr, wr_d, "sync"), (wbd, wbd_d, "gpsimd")):
            P.dma(q, lambda e, t_=t_, d_=d_: e.dma_start(out=t_[:], in_=d_), "const_" + q, writes=["const"])
        P.dma("sync", lambda e: e.dma_start(out=brr[:], in_=br_d.partition_broadcast(128)), "const_sync", writes=["const"])
        P.op("vector", lambda e: e.memset(onecol[:], 1.0), writes=["const"])
        P.op("scalar", lambda e: e.activation(out=lrud[:, :, 0:2], in_=lruc[:, :, 9:11], func=AF.Exp, scale=-1.0),
             reads=["const"], writes=["lrud"])
        P.op("scalar", lambda e: e.activation(out=lrud[:, :, 0:2], in_=lrud[:, :, 0:2], func=AF.Ln, bias=onecol[:, 0:1]),
             reads=["const"], writes=["lrud"])
        P.op("vector", lambda e: e.tensor_scalar(out=lrud[:, :, 2:4], in0=lrud[:, :, 0:2], scalar1=-8.0, scalar2=None, op0=ALU.mult),
             reads=["lrud"], writes=["lrud2"])
        P.op("vector", lambda e: e.tensor_scalar(out=lrud[:, :, 0:2], in0=lrud[:, :, 0:2], scalar1=-4.0, scalar2=None, op0=ALU.mult),
             reads=["lrud", "lrud2"], writes=["lrud"])
        P.op("vector", lambda e: e.tensor_scalar(out=lrud[:, :, 4:8], in0=lruc[:, :, 5:9], scalar1=0.5, scalar2=None, op0=ALU.mult),
             reads=["const", "lrud"], writes=["lrud"])

        def _init_route():
            P.op("vector", lambda e: e.tensor_copy(out=runb[:], in_=ebase[:]), reads=["const"], writes=["runb"])
            P.op("vector", lambda e: e.tensor_scalar(out=ecap[:], in0=ebase[:], scalar1=float(CAP), scalar2=None, op0=ALU.add),
                 reads=["const"], writes=["const"])

        xt = [sb("xt%d" % i, [128, D], F32) for i in range(2)]
        t1 = sb("t1", [128, D], F32)
        hxb = sb("hxb", [128, D], BF16)
        vecA = sb("vecA", [128, D], F32)
        vecB = sb("vecB", [128, D], F32)
        vecC = sb("vecC", [128, D], F32)
        h2T = sb("h2T", [128, 8, 128], F32)
        h2f = sb("h2f", [128, D], F32)
        ecap = sb("ecap", [128, NE], F32)
        stat = sb("stat", [128, 8], F32)
        h0 = sb("h0", [128, 4, 2, NB], F32)
        rt = sb("rt", [128, 256], F32)
        rtb = sb("rtb", [128, 64], BF16)
        runb = sb("runb", [128, NE], F32)
        slots = sb("slots", [128, NTOK // 128, 2], U32)
        wts = sb("wts", [128, NTOK // 128, 2], F32)

        _init_route()

        def bcast_load(dst, src_row, key, q="sync", reads=()):
            P.dma(q, lambda e: e.dma_start(out=dst[:], in_=src_row.partition_broadcast(128)), key, reads=list(reads), writes=[key])

        if True:
            cT = region(0, [128, 8, 3], F32)
            scT = region(1, [128, 8, 3], BF16)
            modsb = region(4, [3, 6 * D], F32)
            bmod3 = region(28, [3, 6 * D], F32)
            wblk = [region(64 + 8 * i, [128, 8, 512], BF16) for i in range(2)]
            P.dma("sync", lambda e: e.dma_start(out=cT, in_=cT_d), "cT", writes=["cT"])
            P.dma("sync", lambda e: e.dma_start(out=bmod3[0:3], in_=bmod_d.partition_broadcast(3)), "bmod3", writes=["bmod3"])
            P.op("scalar", lambda e: e.activation(out=scT, in_=cT, func=AF.Silu), reads=["cT"], writes=["scT"])
            def p0_block(n):
                wb = wblk[n % 2]
                wk = "wblk%d" % (n % 2)
                P.dma("gpsimd", lambda e: e.dma_start(
                    out=wb, in_=wmod_d[:, n * 512:(n + 1) * 512].rearrange("(k p) n -> p k n", p=128)), wk, writes=[wk])
                ps, pk = next_ps()
                for k in range(8):
                    P.op("tensor", lambda e, k=k: e.matmul(ps[0:3, :], lhsT=scT[:, k, :], rhs=wb[:, k, :],
                                                           start=(k == 0), stop=(k == 7)),
                         reads=["scT", wk, pk] if k else ["scT", wk], writes=[pk])
                P.op("vector", lambda e: e.tensor_tensor(out=modsb[0:3, n * 512:(n + 1) * 512], in0=ps[0:3, :],
                                                         in1=bmod3[0:3, n * 512:(n + 1) * 512], op=ALU.add),
                     reads=[pk, "bmod3"], writes=["modsb"])

            for n in range(4):
                p0_block(n)
            P.dma("sync", lambda e: e.dma_start(out=mod_d[:, 0:2 * D], in_=modsb[0:3, 0:2 * D]), "modsbA", reads=["modsb"], writes=["mod_dA"])
            p0_rest = list(range(4, 12))

            def p0_some(k):
                for _ in range(k):
                    if p0_rest:
                        p0_block(p0_rest.pop(0))

        def norm_mod_tile(x, xkey, A, S, dst, dkey, akeys):
            P.op("scalar", lambda e: e.activation(out=t1[:], in_=x[:], func=AF.Square, accum_out=stat[:, 0:1]),
                 reads=[xkey], writes=["t1", "stat"])
            P.op("vector", lambda e: e.tensor_scalar(out=stat[:, 1:2], in0=stat[:, 0:1], scalar1=1.0 / D, scalar2=EPS,
                                                     op0=ALU.mult, op1=ALU.add), reads=["stat"], writes=["stat"])
            P.op("scalar", lambda e: e.activation(out=stat[:, 2:3], in_=stat[:, 1:2], func=AF.Sqrt),
                 reads=["stat"], writes=["stat"])
            P.op("vector", lambda e: e.reciprocal(out=stat[:, 3:4], in_=stat[:, 2:3]), reads=["stat"], writes=["stat"])
            P.op("vector", lambda e: e.scalar_tensor_tensor(out=t1[:], in0=x[:], scalar=stat[:, 3:4], in1=A[:],
                                                            op0=ALU.mult, op1=ALU.mult),
                 reads=[xkey, "stat"] + akeys, writes=["t1"])
            P.op("vector", lambda e: e.tensor_tensor(out=dst[:], in0=t1[:], in1=S[:], op=ALU.add),
                 reads=["t1"] + akeys, writes=[dkey])

        def transpose_bf(src, skey, dstT, dkey):
            ps, pk = next_ps()
            psb = ps[:].bitcast(BF16)
            for k in range(8):
                P.op("tensor", lambda e, k=k: e.transpose(out=psb[:, k * 128:(k + 1) * 128], in_=src[:, k * 128:(k + 1) * 128],
                                                         identity=idb[:]),
                     reads=[skey, "const"] + ([pk] if k else []), writes=[pk])
            P.op("scalar", lambda e: e.activation(out=dstT, in_=psb.rearrange("p (k t) -> p k t", k=8), func=AF.Copy),
                 reads=[pk], writes=[dkey])

        def lru_chunk(u, ukey, Tn, c, init_f, init_b, bufs, okey, nseg=1):
            xc, xcb, rbuf, ig, a, hf, hb = [b[:, 0:Tn] for b in bufs]
            cst = ["const", "lrud", "lrud2"]
            L = Tn // nseg
            P.op("vector", lambda e: e.tensor_scalar(out=xc, in0=u, scalar1=lruc[:, c, 2:3], scalar2=lruc[:, c, 4:5],
                                                     op0=ALU.mult, op1=ALU.add), reads=[ukey] + cst, writes=["xc"])
            xc3 = xc.rearrange("p (s t) -> p s t", s=nseg)
            u3 = u.rearrange("p (s t) -> p s t", s=nseg)
            for (k, o0, o1, i0, i1) in ((0, 2, L, 0, L - 2), (1, 1, L, 0, L - 1), (3, 0, L - 1, 1, L)):
                P.op("vector", lambda e, k=k, o0=o0, o1=o1, i0=i0, i1=i1: e.scalar_tensor_tensor(
                    out=xc3[:, :, o0:o1], in0=u3[:, :, i0:i1], scalar=lruc[:, c, k:k + 1], in1=xc3[:, :, o0:o1],
                    op0=ALU.mult, op1=ALU.add), reads=[ukey, "xc"], writes=["xc"])
            P.op("scalar", lambda e: e.activation(out=xcb, in_=xc, func=AF.Copy), reads=["xc"], writes=["xcb"])
            NH = 2 if (Tn == T and nseg == 1) else 1
            H = Tn // NH
            sets = [(rbuf[:, s0 * H:(s0 + 1) * H], ig[:, s0 * H:(s0 + 1) * H], a[:, s0 * H:(s0 + 1) * H], s0) for s0 in range(NH)] \
                if NH == 2 else [(rbuf, ig, a, 0)]
            its = []
            for d in range(2):
                order = list(range(NH)) if d == 0 else list(range(NH - 1, -1, -1))
                for oi, hh in enumerate(order):
                    its.append((d, hh, oi, sets[len(its) % len(sets)]))

            def keys(si):
                return "rbuf%d" % si, "ig%d" % si, "abuf%d" % si

            def front(itd):
                d, hh, oi, (rb_, ig_, a_, si) = itd
                rk, ik, ak = keys(si)
                c0 = hh * H
                for tb in range(0, H, 512):
                    n = min(512, H - tb)
                    for (g, dst, dk, bcol) in ((0, rb_, rk, 4 + d), (1, ig_, ik, 6 + d)):
                        ps, pk = next_ps()
                        P.op("tensor", lambda e, ps=ps, g=g, tb=tb, n=n: e.matmul(
                            ps[:, 0:n], lhsT=wbd[:, (g * 2 + d) * 4 + c, :], rhs=xcb[:, c0 + tb:c0 + tb + n], start=True, stop=True),
                            reads=["xcb", "const"], writes=[pk])
                        P.op("scalar", lambda e, ps=ps, dst=dst, tb=tb, n=n, bcol=bcol: e.activation(
                            out=dst[:, tb:tb + n], in_=ps[:, 0:n], func=AF.Tanh, scale=0.5, bias=lrud[:, c, bcol:bcol + 1]),
                            reads=[pk] + cst, writes=[dk])
                P.op("scalar", lambda e: e.activation(out=a_, in_=rb_, func=AF.Exp, scale=lrud[:, c, d:d + 1],
                                                      bias=lrud[:, c, d:d + 1]), reads=[rk] + cst, writes=[ak])

            def mid(itd):
                d, hh, oi, (rb_, ig_, a_, si) = itd
                rk, ik, ak = keys(si)
                P.op("vector", lambda e: e.tensor_tensor(out=rb_, in0=a_, in1=a_, op=ALU.mult), reads=[ak, rk], writes=[rk])
                P.op("vector", lambda e: e.tensor_scalar(out=rb_, in0=rb_, scalar1=1.0, scalar2=-1.0, op0=ALU.min, op1=ALU.mult),
                     reads=[rk], writes=[rk])

            def back(itd):
                d, hh, oi, (rb_, ig_, a_, si) = itd
                rk, ik, ak = keys(si)
                c0 = hh * H
                P.op("scalar", lambda e: e.activation(out=rb_, in_=rb_, func=AF.Sqrt, bias=onecol[:, 0:1]),
                     reads=[rk, "const"], writes=[rk])
                P.op("vector", lambda e: e.scalar_tensor_tensor(out=ig_, in0=ig_, scalar=1.0, in1=rb_, op0=ALU.add, op1=ALU.mult),
                     reads=[rk, ik], writes=[ik])
                P.op("vector", lambda e: e.scalar_tensor_tensor(out=ig_, in0=ig_, scalar=0.5, in1=xc[:, c0:c0 + H],
                                                                op0=ALU.mult, op1=ALU.mult), reads=[ik, "xc"], writes=[ik])
                for sg in range(1, nseg):
                    col = sg * L if d == 0 else sg * L - 1
                    P.op("vector", lambda e, col=col: e.memset(a_[:, col:col + 1], 0.0), reads=[ak], writes=[ak])
                if d == 0:
                    ini = init_f if oi == 0 else hf[:, c0 - 1:c0]
                    P.op("vector", lambda e: e.tensor_tensor_scan(
                        out=hf[:, c0:c0 + H], data0=a_, data1=ig_, initial=ini, op0=ALU.mult, op1=ALU.add),
                        reads=[ak, ik, "h0", okey + "f"], writes=[okey + "f"])
                else:
                    ini = init_b if oi == 0 else hb[:, c0 + H:c0 + H + 1]
                    P.op("vector", lambda e: e.tensor_tensor_scan(
                        out=hb[:, c0:c0 + H][:, ::-1], data0=a_[:, ::-1], data1=ig_[:, ::-1], initial=ini,
                        op0=ALU.mult, op1=ALU.add), reads=[ak, ik, "h0", okey + "b"], writes=[okey + "b"])

            front(its[0])
            for k, itd in enumerate(its):
                mid(itd)
                if k + 1 < len(its) and len(sets) > 1:
                    front(its[k + 1])
                back(itd)
                if k + 1 < len(its) and len(sets) == 1:
                    front(its[k + 1])
            return hf, hb

        BIG = 1.0e6
        _bc = {}

        def bc_reg(e):
            if "r" not in _bc:
                _bc["r"] = e.to_reg(NSLOT - 1)
            return _bc["r"]


        def route_tile(tile):
            V = lambda c0, c1: rt[:, c0:c1]
            dv = lambda fn, r=("rt",), w=("rt",): P.op("vector", fn, reads=list(r), writes=list(w))
            for hh in range(2):
                ps, pk = next_ps()
                for k in range(4):
                    kk = hh * 4 + k
                    P.op("tensor", lambda e, ps=ps, k=k, kk=kk: e.transpose(out=ps[:, k * 128:(k + 1) * 128],
                                                                         in_=h2f[:, kk * 128:(kk + 1) * 128], identity=idf[:]),
                         reads=["h2f", "const"] + ([pk] if k else []), writes=[pk])
                P.op("scalar", lambda e, ps=ps, hh=hh: e.activation(
                    out=h2T[:, hh * 4:(hh + 1) * 4, :], in_=ps[:, :].rearrange("p (k t) -> p k t", k=4), func=AF.Copy),
                    reads=[pk], writes=["h2T"])
            ps, pk = next_ps()
            for k in range(8):
                P.op("tensor", lambda e, ps=ps, k=k: e.matmul(ps[:, 0:36], lhsT=h2T[:, k, :], rhs=wr[:, k, :],
                                                              start=(k == 0), stop=(k == 7)),
                     reads=["h2T", "const"] + ([pk] if k else []), writes=[pk])
            lg = V(0, 36)
            dv(lambda e, ps=ps: e.tensor_tensor(out=lg, in0=ps[:, 0:36], in1=brr[:, :], op=ALU.add), r=[pk, "const", "rt"])
            dv(lambda e: e.tensor_reduce(out=V(36, 37), in_=V(0, 4), axis=AX.X, op=ALU.max))
            dv(lambda e: e.tensor_scalar(out=V(40, 44), in0=V(0, 4), scalar1=V(36, 37), scalar2=None, op0=ALU.is_equal))
            dv(lambda e: e.tensor_scalar(out=V(37, 38), in0=V(36, 37), scalar1=-1.0, scalar2=None, op0=ALU.mult))
            P.op("scalar", lambda e: e.activation(out=V(44, 48), in_=V(0, 4), func=AF.Exp, bias=V(37, 38), accum_out=V(38, 39)),
                 reads=["rt"], writes=["rt"])
            dv(lambda e: e.reciprocal(out=V(39, 40), in_=V(38, 39)))
            dv(lambda e: e.tensor_scalar(out=V(48, 56), in0=V(4, 12), scalar1=V(40, 41), scalar2=None, op0=ALU.mult))
            for g in range(1, 4):
                dv(lambda e, g=g: e.scalar_tensor_tensor(out=V(48, 56), in0=V(4 + 8 * g, 12 + 8 * g), scalar=V(40 + g, 41 + g),
                                                         in1=V(48, 56), op0=ALU.mult, op1=ALU.add))
            dv(lambda e: e.max(out=V(56, 64), in_=V(48, 56)))
            dv(lambda e: e.tensor_scalar(out=V(64, 72), in0=V(48, 56), scalar1=V(56, 57), scalar2=None, op0=ALU.is_equal))
            dv(lambda e: e.tensor_scalar(out=V(72, 80), in0=V(48, 56), scalar1=V(57, 58), scalar2=None, op0=ALU.is_equal))
            dv(lambda e: e.tensor_tensor(out=V(80, 81), in0=V(57, 58), in1=V(56, 57), op=ALU.subtract))
            P.op("scalar", lambda e: e.activation(out=V(81, 82), in_=V(80, 81), func=AF.Exp), reads=["rt"], writes=["rt"])
            dv(lambda e: e.tensor_scalar(out=V(82, 83), in0=V(81, 82), scalar1=1.0, scalar2=None, op0=ALU.add))
            dv(lambda e: e.reciprocal(out=V(83, 84), in_=V(82, 83)))
            dv(lambda e: e.tensor_tensor(out=V(84, 85), in0=V(83, 84), in1=V(39, 40), op=ALU.mult))
            dv(lambda e: e.tensor_tensor(out=V(85, 86), in0=V(84, 85), in1=V(81, 82), op=ALU.mult))
            for g in range(4):
                dv(lambda e, g=g: e.tensor_scalar(out=V(96 + 8 * g, 104 + 8 * g), in0=V(64, 72), scalar1=V(40 + g, 41 + g),
                                                  scalar2=None, op0=ALU.mult))
                dv(lambda e, g=g: e.tensor_scalar(out=V(128 + 8 * g, 136 + 8 * g), in0=V(72, 80), scalar1=V(40 + g, 41 + g),
                                                  scalar2=None, op0=ALU.mult))
            dv(lambda e: e.tensor_tensor(out=rtb[:, 0:32], in0=V(96, 128), in1=V(128, 160), op=ALU.add), w=["rtb"])
            ps2, pk2 = next_ps()
            P.op("tensor", lambda e, ps2=ps2: e.matmul(ps2[:, 0:32], lhsT=ltri[:, :], rhs=rtb[:, 0:32], start=True, stop=True),
                 reads=["rtb", "const"], writes=[pk2])
            P.op("tensor", lambda e, ps2=ps2: e.matmul(ps2[:, 32:64], lhsT=ones[:, :], rhs=rtb[:, 0:32], start=True, stop=True),
                 reads=["rtb", "const", pk2], writes=[pk2])
            dv(lambda e, ps2=ps2: e.tensor_tensor(out=V(160, 192), in0=ps2[:, 0:32], in1=runb[:, :], op=ALU.add),
               r=[pk2, "runb", "rt"])
            dv(lambda e, ps2=ps2: e.tensor_tensor(out=runb[:, :], in0=ps2[:, 32:64], in1=runb[:, :], op=ALU.add),
               r=[pk2, "runb", "rt"], w=["runb"])
            dv(lambda e: e.tensor_tensor(out=V(192, 224), in0=V(160, 192), in1=ecap[:, :], op=ALU.is_lt), r=["rt", "const"])
            for k, (oh0, wcol) in enumerate(((96, 84), (128, 85))):
                dv(lambda e, oh0=oh0: e.tensor_tensor(out=V(oh0, oh0 + 32), in0=V(oh0, oh0 + 32), in1=V(192, 224), op=ALU.mult))
                dv(lambda e, oh0=oh0: e.tensor_tensor(out=V(224, 256), in0=V(oh0, oh0 + 32), in1=V(160, 192), op=ALU.mult))
                dv(lambda e: e.tensor_reduce(out=V(86, 87), in_=V(224, 256), axis=AX.X, op=ALU.add))
                dv(lambda e, oh0=oh0: e.tensor_reduce(out=V(87, 88), in_=V(oh0, oh0 + 32), axis=AX.X, op=ALU.add))
                dv(lambda e: e.scalar_tensor_tensor(out=V(88, 89), in0=V(87, 88), scalar=-BIG, in1=V(86, 87),
                                                    op0=ALU.mult, op1=ALU.add))
                dv(lambda e: e.tensor_scalar(out=V(88, 89), in0=V(88, 89), scalar1=BIG, scalar2=None, op0=ALU.add))
                dv(lambda e, k=k: e.tensor_copy(out=slots[:, tile, k:k + 1], in_=V(88, 89)), w=["slots", "rt"])
                dv(lambda e, k=k, wcol=wcol: e.tensor_tensor(out=wts[:, tile, k:k + 1], in0=V(wcol, wcol + 1), in1=V(87, 88),
                                                             op=ALU.mult), w=["wts", "rt"])
                P.dma("gpsimd", lambda e, k=k: e.indirect_dma_start(
                    out=xs_d[:, :], out_offset=bass.IndirectOffsetOnAxis(ap=slots[:, tile, k:k + 1], axis=0),
                    in_=hxb[:, :], in_offset=None, bounds_check=bc_reg(e), oob_is_err=False),
                    "scat", reads=["slots", "hxb"], writes=["xs_d"])

        def router_logits(i, lg_all, h2b_all):
            P.op("scalar", lambda e: e.activation(out=h2b_all[:, i, :], in_=h2f[:], func=AF.Copy), reads=["h2f"], writes=["h2b_all"])
            for hh in range(2):
                ps, pk = next_ps()
                for k in range(4):
                    kk = hh * 4 + k
                    P.op("tensor", lambda e, ps=ps, k=k, kk=kk: e.transpose(out=ps[:, k * 128:(k + 1) * 128],
                                                                         in_=h2f[:, kk * 128:(kk + 1) * 128], identity=idf[:]),
                         reads=["h2f", "const"] + ([pk] if k else []), writes=[pk])
                P.op("scalar", lambda e, ps=ps, hh=hh: e.activation(
                    out=h2T[:, hh * 4:(hh + 1) * 4, :], in_=ps[:, :].rearrange("p (k t) -> p k t", k=4), func=AF.Copy),
                    reads=[pk], writes=["h2T"])
            ps, pk = next_ps()
            for k in range(8):
                P.op("tensor", lambda e, ps=ps, k=k: e.matmul(ps[:, 0:36], lhsT=h2T[:, k, :], rhs=wr[:, k, :],
                                                              start=(k == 0), stop=(k == 7)),
                     reads=["h2T", "const"] + ([pk] if k else []), writes=[pk])
            P.op("vector", lambda e, ps=ps: e.tensor_tensor(out=lg_all[:, i, :], in0=ps[:, 0:36], in1=brr[:, :], op=ALU.add),
                 reads=[pk, "const"], writes=["lg_all"])

        def route_batch(b, lg_all, h2b_all, RS, RB, runall):
            NT_ = T // 128
            V = lambda c0, c1: RS[:, :, c0:c1]
            bc = lambda ap, w: ap.to_broadcast([128, NT_, w])
            dv = lambda fn, r=("rs",), w=("rs",): P.op("vector", fn, reads=list(r), writes=list(w))
            dv(lambda e: e.tensor_reduce(out=V(36, 37), in_=lg_all[:, :, 0:4], axis=AX.X, op=ALU.max), r=["lg_all", "rs"])
            dv(lambda e: e.tensor_tensor(out=V(40, 44), in0=lg_all[:, :, 0:4], in1=bc(V(36, 37), 4), op=ALU.is_equal), r=["lg_all", "rs"])
            dv(lambda e: e.tensor_tensor(out=V(44, 48), in0=lg_all[:, :, 0:4], in1=bc(V(36, 37), 4), op=ALU.subtract), r=["lg_all", "rs"])
            P.op("scalar", lambda e: e.activation(out=V(44, 48), in_=V(44, 48), func=AF.Exp), reads=["rs"], writes=["rs"])
            dv(lambda e: e.tensor_reduce(out=V(38, 39), in_=V(44, 48), axis=AX.X, op=ALU.add))
            dv(lambda e: e.reciprocal(out=V(39, 40), in_=V(38, 39)))
            t48 = RS[:, :, 96:128].rearrange("p i (g j) -> p i g j", g=4)
            dv(lambda e: e.tensor_tensor(out=t48, in0=V(40, 44).unsqueeze(3).to_broadcast([128, NT_, 4, 8]),
                                         in1=lg_all[:, :, 4:36].rearrange("p i (g j) -> p i g j", g=4), op=ALU.mult), r=["lg_all", "rs"])
            dv(lambda e: e.tensor_reduce(out=V(48, 56), in_=t48.rearrange("p i g j -> p i j g"), axis=AX.X, op=ALU.add))
            dv(lambda e: e.tensor_reduce(out=V(91, 92), in_=V(48, 56), axis=AX.X, op=ALU.max))
            dv(lambda e: e.tensor_tensor(out=V(104, 112), in0=V(48, 56), in1=bc(V(91, 92), 8), op=ALU.is_equal))
            dv(lambda e: e.scalar_tensor_tensor(out=V(96, 104), in0=V(104, 112), scalar=-BIG, in1=V(48, 56), op0=ALU.mult, op1=ALU.add))
            dv(lambda e: e.tensor_reduce(out=V(92, 93), in_=V(96, 104), axis=AX.X, op=ALU.max))
            dv(lambda e: e.tensor_tensor(out=V(112, 120), in0=V(96, 104), in1=bc(V(92, 93), 8), op=ALU.is_equal))
            dv(lambda e: e.tensor_tensor(out=V(93, 94), in0=V(92, 93), in1=V(91, 92), op=ALU.subtract))
            P.op("scalar", lambda e: e.activation(out=V(93, 94), in_=V(93, 94), func=AF.Exp), reads=["rs"], writes=["rs"])
            dv(lambda e: e.tensor_scalar(out=V(94, 95), in0=V(93, 94), scalar1=1.0, scalar2=None, op0=ALU.add))
            dv(lambda e: e.reciprocal(out=V(94, 95), in_=V(94, 95)))
            dv(lambda e: e.tensor_tensor(out=V(95, 96), in0=V(94, 95), in1=V(39, 40), op=ALU.mult))
            dv(lambda e: e.tensor_tensor(out=V(37, 38), in0=V(95, 96), in1=V(93, 94), op=ALU.mult))
            for (o0, mcol) in ((128, 104), (160, 112)):
                dv(lambda e, o0=o0, mcol=mcol: e.tensor_tensor(
                    out=RS[:, :, o0:o0 + 32].rearrange("p i (g j) -> p i g j", g=4),
                    in0=V(40, 44).unsqueeze(3).to_broadcast([128, NT_, 4, 8]),
                    in1=V(mcol, mcol + 8).unsqueeze(2).to_broadcast([128, NT_, 4, 8]), op=ALU.mult))
            dv(lambda e: e.tensor_tensor(out=RB[:, :, :], in0=V(128, 160), in1=V(160, 192), op=ALU.add), w=["rb"])
            psp, kp = next_ps()
            pst, kt = next_ps()
            rbf = RB[:, :, :].rearrange("p i e -> p (i e)")
            P.op("tensor", lambda e: e.matmul(psp[:, :], lhsT=ltri[:, :], rhs=rbf, start=True, stop=True), reads=["rb", "const"], writes=[kp])
            P.op("tensor", lambda e: e.matmul(pst[:, :], lhsT=ones[:, :], rhs=rbf, start=True, stop=True), reads=["rb", "const"], writes=[kt])
            dv(lambda e: e.tensor_copy(out=runall[:, 0, :], in_=runb[:, :]), r=["runb"], w=["runall"])
            for i in range(NT_):
                dv(lambda e, i=i: e.tensor_tensor(out=runall[:, i + 1, :], in0=runall[:, i, :], in1=pst[:, i * 32:(i + 1) * 32], op=ALU.add),
                   r=["runall", kt], w=["runall"])
            dv(lambda e: e.tensor_copy(out=runb[:, :], in_=runall[:, NT_, :]), r=["runall"], w=["runb"])
            dv(lambda e: e.tensor_tensor(out=V(192, 224), in0=psp[:, :].rearrange("p (i e) -> p i e", e=32), in1=runall[:, 0:NT_, :], op=ALU.add),
               r=[kp, "runall", "rs"])
            dv(lambda e: e.tensor_tensor(out=V(224, 256), in0=V(192, 224), in1=ecap[:, :].unsqueeze(1).to_broadcast([128, NT_, 32]), op=ALU.is_lt),
               r=["rs", "const"])
            t0_ = b * NT_
            for k, (o0, wcol) in enumerate(((128, 95), (160, 37))):
                dv(lambda e, o0=o0: e.tensor_tensor(out=V(o0, o0 + 32), in0=V(o0, o0 + 32), in1=V(224, 256), op=ALU.mult))
                dv(lambda e, o0=o0: e.tensor_tensor(out=V(56, 88), in0=V(o0, o0 + 32), in1=V(192, 224), op=ALU.mult))
                dv(lambda e: e.tensor_reduce(out=V(88, 89), in_=V(56, 88), axis=AX.X, op=ALU.add))
                dv(lambda e, o0=o0: e.tensor_reduce(out=V(89, 90), in_=V(o0, o0 + 32), axis=AX.X, op=ALU.add))
                dv(lambda e: e.scalar_tensor_tensor(out=V(90, 91), in0=V(89, 90), scalar=-BIG, in1=V(88, 89), op0=ALU.mult, op1=ALU.add))
                dv(lambda e: e.tensor_scalar(out=V(90, 91), in0=V(90, 91), scalar1=BIG, scalar2=None, op0=ALU.add))
                dv(lambda e, k=k: e.tensor_copy(out=slots[:, t0_:t0_ + NT_, k:k + 1], in_=V(90, 91)), w=["slots", "rs"])
                dv(lambda e, k=k, wcol=wcol: e.tensor_tensor(out=wts[:, t0_:t0_ + NT_, k:k + 1], in0=V(wcol, wcol + 1), in1=V(89, 90), op=ALU.mult),
                   w=["wts", "rs"])
            for i in range(NT_):
                for k in range(2):
                    P.dma("gpsimd", lambda e, i=i, k=k: e.indirect_dma_start(
                        out=xs_d[:, :], out_offset=bass.IndirectOffsetOnAxis(ap=slots[:, t0_ + i, k:k + 1], axis=0),
                        in_=h2b_all[:, i, :], in_offset=None, bounds_check=bc_reg(e), oob_is_err=False),
                        "scat", reads=["slots", "h2b_all"], writes=["xs_d"])

        def lru_bufs(off_kb, Tn=T):
            return [region(off_kb, [128, Tn], F32), region(off_kb + 8, [128, Tn], BF16), region(off_kb + 12, [128, Tn], F32),
                    region(off_kb + 20, [128, Tn], F32), region(off_kb + 28, [128, Tn], F32), region(off_kb + 36, [128, Tn], F32),
                    region(off_kb + 44, [128, Tn], F32)]

        def dbg_store(name, src, skey, shape, dt=F32):
            o = dout(name, shape, dt)
            P.dma("sync", lambda e: e.dma_start(out=o, in_=src), "dbg_" + name, reads=[skey], writes=["dbg_" + name])

        if True:
            bcast_load(vecC, n1g_d[0:1, :], "vecC")
            bcast_load(vecA, mod_d[2:3, D:2 * D], "vecA", reads=["mod_dA"])
            bcast_load(vecB, mod_d[2:3, 0:D], "vecB", reads=["mod_dA"])
            P.op("vector", lambda e: e.scalar_tensor_tensor(out=vecA[:], in0=vecA[:], scalar=1.0, in1=vecC[:],
                                                            op0=ALU.add, op1=ALU.mult), reads=["vecA", "vecC"], writes=["vecA"])
            hcT = region(84, [128, 8, 512], BF16)
            winR = region(92, [128, 8, 512], BF16)
            uct = region(100, [128, 4, 512], F32)
            lb = lru_bufs(108, NB * TC)
            P.dma("gpsimd", lambda e: e.dma_start(out=winR, in_=win_d[:, 512:1024].rearrange("(k p) n -> p k n", p=128)),
                  "winR", writes=["winR"])
            for i in range(4):
                xb = xt[i % 2]
                xk = "xt%d" % (i % 2)
                P.dma("sync", lambda e, xb=xb, i=i: e.dma_start(out=xb[:], in_=ctx_d[i * 128:(i + 1) * 128, :]), xk, writes=[xk])
                norm_mod_tile(xb, xk, vecA, vecB, hxb, "hxb", ["vecA", "vecB"])
                transpose_bf(hxb, "hxb", hcT[:, :, i * 128:(i + 1) * 128], "hcT")
            p0_some(2)
            for c in range(4):
                ps, pk = next_ps()
                for k in range(8):
                    P.op("tensor", lambda e, ps=ps, c=c, k=k: e.matmul(ps[:, :], lhsT=winR[:, k, c * 128:(c + 1) * 128],
                                                                      rhs=hcT[:, k, :], start=(k == 0), stop=(k == 7)),
                         reads=["winR", "hcT"] + ([pk] if k else []), writes=[pk])
                P.op("scalar", lambda e, ps=ps, c=c: e.activation(out=uct[:, c, :], in_=ps[:, :], func=AF.Copy),
                     reads=[pk], writes=["uct"])
            for c in range(4):
                hf, hb = lru_chunk(uct[:, c, :], "uct", NB * TC, c, 0.0, 0.0, lb, "hc", nseg=NB)
                P.op("vector", lambda e, hf=hf, c=c: e.tensor_copy(
                    out=h0[:, c, 0, :], in_=hf.rearrange("p (s t) -> p s t", s=NB)[:, :, TC - 1]), reads=["hcf"], writes=["h0"])
                P.op("vector", lambda e, hb=hb, c=c: e.tensor_copy(
                    out=h0[:, c, 1, :], in_=hb.rearrange("p (s t) -> p s t", s=NB)[:, :, 0]), reads=["hcb"], writes=["h0"])
                p0_some(2)
            p0_some(8)
            P.dma("sync", lambda e: e.dma_start(out=mod_d[:, 2 * D:6 * D], in_=modsb[0:3, 2 * D:6 * D]), "modsbB", reads=["modsb"], writes=["mod_d"])
            if "h0" in dbg:
                dbg_store("h0", h0[:], "h0", [128, 4, 2, NB])
            P.barrier()

        hxT = region(0, [128, 8, T], BF16)
        ufT = region(32, [128, 4, T], BF16)
        gyT = region(48, [128, 4, T], BF16)
        urT = region(64, [128, 4, T], F32)
        winA = region(96, [128, 8, 1536], BF16)
        for b in range(NB if "stop_p1" not in dbg else 0):
            bcast_load(vecC, n1g_d[0:1, :], "vecC")
            bcast_load(vecA, mod_d[b:b + 1, D:2 * D], "vecA")
            bcast_load(vecB, mod_d[b:b + 1, 0:D], "vecB")
            P.op("vector", lambda e: e.scalar_tensor_tensor(out=vecA[:], in0=vecA[:], scalar=1.0, in1=vecC[:],
                                                            op0=ALU.add, op1=ALU.mult), reads=["vecA", "vecC"], writes=["vecA"])
            if b == 0:
                P.dma("gpsimd", lambda e: e.dma_start(out=winA, in_=win_d[:, 0:1536].rearrange("(k p) n -> p k n", p=128)),
                      "winA", writes=["winA"])
            xs1 = [(xt[0], "xt0"), (xt[1], "xt1"), (region(120, [128, D], F32), "xt2")]
            ts1 = [(t1, "t1"), (region(124, [128, D], F32), "t1b")]
            hs1 = [(hxb[:], "hxb"), (h2f[:, 0:D // 2].bitcast(BF16), "h2f")]

            def s1_load(i):
                xb, xk = xs1[i % 3]
                r0 = b * T + i * 128
                P.dma("sync", lambda e: e.dma_start(out=xb[:], in_=x_d[r0:r0 + 128, :]), xk, writes=[xk])

            def s1_stat(i):
                xb, xk = xs1[i % 3]
                tt, tk = ts1[i % 2]
                sc, sk = 4 * (i % 2), "stat%d" % (i % 2)
                P.op("scalar", lambda e: e.activation(out=tt[:], in_=xb[:], func=AF.Square, accum_out=stat[:, sc:sc + 1]),
                     reads=[xk], writes=[tk, sk])
                P.op("vector", lambda e: e.tensor_scalar(out=stat[:, sc + 1:sc + 2], in0=stat[:, sc:sc + 1], scalar1=1.0 / D, scalar2=EPS,
                                                         op0=ALU.mult, op1=ALU.add), reads=[sk], writes=[sk])
                P.op("scalar", lambda e: e.activation(out=stat[:, sc + 2:sc + 3], in_=stat[:, sc + 1:sc + 2], func=AF.Sqrt),
                     reads=[sk], writes=[sk])
                P.op("vector", lambda e: e.reciprocal(out=stat[:, sc + 3:sc + 4], in_=stat[:, sc + 2:sc + 3]), reads=[sk], writes=[sk])

            def s1_apply(i):
                xb, xk = xs1[i % 3]
                tt, tk = ts1[i % 2]
                hb_, hk = hs1[i % 2]
                sc, sk = 4 * (i % 2), "stat%d" % (i % 2)
                P.op("vector", lambda e: e.scalar_tensor_tensor(out=tt[:], in0=xb[:], scalar=stat[:, sc + 3:sc + 4], in1=vecA[:],
                                                                op0=ALU.mult, op1=ALU.mult), reads=[xk, sk, "vecA", tk], writes=[tk])
                P.op("vector", lambda e: e.tensor_tensor(out=hb_, in0=tt[:], in1=vecB[:], op=ALU.add), reads=[tk, "vecB"], writes=[hk])
                transpose_bf(hb_, hk, hxT[:, :, i * 128:(i + 1) * 128], "hxT")

            NT1 = T // 128
            s1_load(0)
            s1_load(1)
            s1_stat(0)
            for i in range(NT1):
                if i + 2 < NT1:
                    s1_load(i + 2)
                if i + 1 < NT1:
                    s1_stat(i + 1)
                s1_apply(i)
            for j in range(12):
                for tb in range(4):
                    ps, pk = next_ps()
                    for k in range(8):
                        P.op("tensor", lambda e, ps=ps, j=j, tb=tb, k=k: e.matmul(
                            ps[:, :], lhsT=winA[:, k, j * 128:(j + 1) * 128], rhs=hxT[:, k, tb * 512:(tb + 1) * 512],
                            start=(k == 0), stop=(k == 7)), reads=["winA", "hxT"] + ([pk] if k else []), writes=[pk])
                    ts = slice(tb * 512, (tb + 1) * 512)
                    if j < 4:
                        P.op("scalar", lambda e, ps=ps, j=j, ts=ts: e.activation(out=ufT[:, j, ts], in_=ps[:, :], func=AF.Copy),
                             reads=[pk], writes=["ufT"])
                    elif j < 8:
                        P.op("vector", lambda e, ps=ps, j=j, ts=ts: e.tensor_copy(out=urT[:, j - 4, ts], in_=ps[:, :]),
                             reads=[pk], writes=["urT"])
                    else:
                        P.op("scalar", lambda e, ps=ps, j=j, ts=ts: e.activation(out=gyT[:, j - 8, ts], in_=ps[:, :], func=AF.Gelu),
                             reads=[pk], writes=["gyT"])
            if "proj" in dbg and b == 0:
                dbg_store("ufT", ufT, "ufT", [128, 4, T], BF16)
                dbg_store("urT", urT, "urT", [128, 4, T])
                dbg_store("gyT", gyT, "gyT", [128, 4, T], BF16)
            P.barrier()
            lb = lru_bufs(96)
            if "lru" in dbg and b == 0:
                o_hf = dout("hf", [128, 4, T])
                o_hb = dout("hb", [128, 4, T])
            for c in range(4):
                hf, hb = lru_chunk(urT[:, c, :], "urT", T, c, h0[:, c, 0, b:b + 1], h0[:, c, 1, b:b + 1], lb, "hl")
                if "lru" in dbg and b == 0:
                    P.dma("sync", lambda e, hf=hf, c=c: e.dma_start(out=o_hf[:, c, :], in_=hf), "dbg_hf", reads=["hlf"], writes=["dbg_hf"])
                    P.dma("sync", lambda e, hb=hb, c=c: e.dma_start(out=o_hb[:, c, :], in_=hb), "dbg_hb", reads=["hlb"], writes=["dbg_hb"])
                P.op("vector", lambda e, hf=hf, hb=hb: e.tensor_tensor(out=hf, in0=hf, in1=hb, op=ALU.add),
                     reads=["hlf", "hlb"], writes=["hlf"])
                P.op("vector", lambda e, hf=hf, c=c: e.tensor_tensor(out=gyT[:, c, :], in0=hf, in1=gyT[:, c, :], op=ALU.mult),
                     reads=["hlf", "gyT"], writes=["gyT"])
            P.barrier()
            if "stop_s4" in dbg:
                break
            Adft = region(64, [128, 16, 2, 4, 128], BF16)
            cpb = [region(96 + 8 * i, [128, 16, 256], BF16) for i in range(2)]
            spb = [region(112 + 8 * i, [128, 16, 256], BF16) for i in range(2)]
            fmT = region(128, [128, 4, T], BF16)
            fmtok = [region(144 + 1 * i, [128, 512], BF16) for i in range(2)]
            ev = 0
            for j in range(16):
                for half in range(2):
                    ps, pk = next_ps()
                    for gg in range(2):
                        g = 2 * half + gg
                        P.op("tensor", lambda e, ps=ps, g=g, gg=gg, j=j: e.matmul(
                            ps[:, gg * 256:(gg + 1) * 256], lhsT=ufT[:, g, j * 128:(j + 1) * 128], rhs=cs[:, :],
                            start=True, stop=True), reads=["ufT", "const"] + ([pk] if gg else []), writes=[pk])
                    dst = Adft[:, j, :, 2 * half:2 * half + 2, :]
                    src = ps[:, :].rearrange("p (g c x) -> p c g x", g=2, c=2)
                    if ev % 2 == 0:
                        P.op("scalar", lambda e, src=src, dst=dst: e.activation(out=dst, in_=src, func=AF.Copy),
                             reads=[pk], writes=["Adft"])
                    else:
                        P.op("vector", lambda e, src=src, dst=dst: e.tensor_copy(out=dst, in_=src), reads=[pk], writes=["Adft"])
                    ev += 1
            for n in range(8):
                cb, sbf = cpb[n % 2], spb[n % 2]
                ck, sk = "cpb%d" % (n % 2), "spb%d" % (n % 2)
                P.dma("sync", lambda e, cb=cb, n=n: e.dma_start(
                    out=cb, in_=cp_d[:, n * 256:(n + 1) * 256].rearrange("(j p) n -> p j n", p=128)), ck, writes=[ck])
                P.dma("sync", lambda e, sbf=sbf, n=n: e.dma_start(
                    out=sbf, in_=spn_d[:, n * 256:(n + 1) * 256].rearrange("(j p) n -> p j n", p=128)), sk, writes=[sk])
                for mm in range(2):
                    m = 2 * n + mm
                    ps, pk = next_ps()
                    for j in range(16):
                        P.op("tensor", lambda e, ps=ps, j=j, cb=cb, mm=mm: e.matmul(
                            ps[:, :], lhsT=cb[:, j, mm * 128:(mm + 1) * 128], rhs=Adft[:, j, 0, :, :].rearrange("p g x -> p (g x)"),
                            start=(j == 0), stop=False), reads=["Adft", ck] + ([pk] if j else []), writes=[pk])
                        P.op("tensor", lambda e, ps=ps, j=j, sbf=sbf, mm=mm: e.matmul(
                            ps[:, :], lhsT=sbf[:, j, mm * 128:(mm + 1) * 128], rhs=Adft[:, j, 1, :, :].rearrange("p g x -> p (g x)"),
                            start=False, stop=(j == 15)), reads=["Adft", sk, pk], writes=[pk])
                    ft, fk = fmtok[m % 2], "fmtok%d" % (m % 2)
                    P.op("scalar", lambda e, ps=ps, ft=ft: e.activation(out=ft[:, :], in_=ps[:, :], func=AF.Copy), reads=[pk], writes=[fk])
                    ps2, pk2 = next_ps()
                    psb2 = ps2[:].bitcast(BF16)
                    for g in range(4):
                        P.op("tensor", lambda e, psb2=psb2, ft=ft, g=g: e.transpose(
                            out=psb2[:, g * 128:(g + 1) * 128], in_=ft[:, g * 128:(g + 1) * 128], identity=idb[:]),
                            reads=[fk, "const"] + ([pk2] if g else []), writes=[pk2])
                    P.op("vector", lambda e, psb2=psb2, m=m: e.tensor_copy(
                        out=fmT[:, :, m * 128:(m + 1) * 128], in_=psb2[:, 0:512].rearrange("p (g t) -> p g t", g=4)),
                        reads=[pk2], writes=["fmT"])
            if "fm" in dbg and b == 0:
                dbg_store("fmT", fmT, "fmT", [128, 4, T], BF16)
            P.barrier()
            mT = region(64, [128, 8, T], BF16)
            winB = region(96, [128, 8, 2048], BF16)
            wfo = region(144, [128, 4, D], BF16)
            wlo = region(152, [128, 4, D], BF16)
            wout = region(32, [128, 8, D], BF16)
            P.dma("gpsimd", lambda e: e.dma_start(out=winB, in_=win_d[:, 1536:3584].rearrange("(k p) n -> p k n", p=128)),
                  "winB", writes=["winB"])
            P.dma("gpsimd", lambda e: e.dma_start(out=wfo, in_=wfo_d.rearrange("(k p) n -> p k n", p=128)), "wfo", writes=["wfo"])
            P.dma("gpsimd", lambda e: e.dma_start(out=wlo, in_=wlo_d.rearrange("(k p) n -> p k n", p=128)), "wlo", writes=["wlo"])
            P.dma("gpsimd", lambda e: e.dma_start(out=wout, in_=wout_d.rearrange("(k p) n -> p k n", p=128)), "wout", writes=["wout"])
            tmps = [(t1, "t1"), (xt[0], "xt0"), (xt[1], "xt1")]
            it = 0
            for dc in range(8):
                dsl = slice(dc * 128, (dc + 1) * 128)
                for tb in range(4):
                    ts = slice(tb * 512, (tb + 1) * 512)
                    tmp, tk = tmps[it % 3]
                    it += 1
                    pss = []
                    for (wt_, wk_, off, src, sk_, nk) in ((wfo, "wfo", 0, fmT, "fmT", 4), (winB, "winB", 0, hxT, "hxT", 8),
                                                          (wlo, "wlo", 0, gyT, "gyT", 4), (winB, "winB", 1024, hxT, "hxT", 8)):
                        ps, pk = next_ps()
                        for k in range(nk):
                            P.op("tensor", lambda e, ps=ps, wt_=wt_, off=off, src=src, k=k, dc=dc, ts=ts, nk=nk: e.matmul(
                                ps[:, :], lhsT=wt_[:, k, off + dc * 128:off + (dc + 1) * 128], rhs=src[:, k, ts],
                                start=(k == 0), stop=(k == nk - 1)), reads=[wk_, sk_] + ([pk] if k else []), writes=[pk])
                        pss.append((ps, pk))
                    (pbf, kbf), (pgf, kgf), (pbr, kbr), (pgr, kgr) = pss
                    P.op("scalar", lambda e, tmp=tmp, pgf=pgf: e.activation(out=tmp[:, 0:512], in_=pgf[:, :], func=AF.Sigmoid),
                         reads=[kgf], writes=[tk])
                    P.op("scalar", lambda e, tmp=tmp, pgr=pgr: e.activation(out=tmp[:, 512:1024], in_=pgr[:, :], func=AF.Sigmoid),
                         reads=[kgr], writes=[tk])
                    P.op("vector", lambda e, tmp=tmp, pbf=pbf: e.tensor_tensor(out=tmp[:, 0:512], in0=tmp[:, 0:512], in1=pbf[:, :],
                                                                             op=ALU.mult), reads=[tk, kbf], writes=[tk])
                    P.op("vector", lambda e, tmp=tmp, pbr=pbr: e.tensor_tensor(out=tmp[:, 512:1024], in0=tmp[:, 512:1024], in1=pbr[:, :],
                                                                             op=ALU.mult), reads=[tk, kbr], writes=[tk])
                    P.op("gpsimd", lambda e, tmp=tmp, dc=dc, ts=ts: e.tensor_tensor(out=mT[:, dc, ts], in0=tmp[:, 0:512],
                                                                                  in1=tmp[:, 512:1024], op=ALU.add),
                         reads=[tk], writes=["mT"])
            P.barrier()
            h2b_all = region(128, [128, T // 128, D], BF16)
            RS = region(0, [128, T // 128, 256], F32)
            RB = region(16, [128, T // 128, 32], BF16)
            lg_all = region(17, [128, T // 128, 36], F32)
            runall = region(20, [128, T // 128 + 1, 32], F32)
            bcast_load(vecA, mod_d[b:b + 1, 2 * D:3 * D], "vecA")
            bcast_load(vecB, mod_d[b:b + 1, 4 * D:5 * D], "vecB")
            bcast_load(vecC, n2g_d[0:1, :], "vecC")
            P.op("vector", lambda e: e.scalar_tensor_tensor(out=vecB[:], in0=vecB[:], scalar=1.0, in1=vecC[:],
                                                            op0=ALU.add, op1=ALU.mult), reads=["vecB", "vecC"], writes=["vecB"])
            bcast_load(vecC, mod_d[b:b + 1, 3 * D:4 * D], "vecC")
            tA = region(48, [128, D], F32)

            def op_mm(i):
                xb, xk = xt[i % 2], "xt%d" % (i % 2)
                r0 = b * T + i * 128
                P.dma("sync", lambda e: e.dma_start(out=xb[:], in_=x_d[r0:r0 + 128, :]), xk, writes=[xk])
                res = []
                for h in range(2):
                    ps, pk = next_ps()
                    for k in range(8):
                        P.op("tensor", lambda e, ps=ps, k=k, h=h: e.matmul(
                            ps[:, :], lhsT=mT[:, k, i * 128:(i + 1) * 128], rhs=wout[:, k, h * 512:(h + 1) * 512],
                            start=(k == 0), stop=(k == 7)), reads=["mT", "wout"] + ([pk] if k else []), writes=[pk])
                    res.append((ps, pk))
                return res

            def op_resid(i, res):
                xb, xk = xt[i % 2], "xt%d" % (i % 2)
                r0 = b * T + i * 128
                for h, (ps, pk) in enumerate(res):
                    P.op("vector", lambda e, ps=ps, h=h: e.tensor_tensor(out=tA[:, h * 512:(h + 1) * 512], in0=ps[:, :],
                                                                       in1=vecA[:, h * 512:(h + 1) * 512], op=ALU.mult),
                         reads=[pk, "vecA"], writes=["tA"])
                P.op("vector", lambda e: e.tensor_tensor(out=xb[:], in0=xb[:], in1=tA[:], op=ALU.add), reads=[xk, "tA"], writes=[xk])
                P.dma("sync", lambda e: e.dma_start(out=x1_d[r0:r0 + 128, :], in_=xb[:]), "x1st%d" % (i % 2),
                      reads=[xk], writes=["x1_d"])

            NT_ = T // 128
            res_next = op_mm(0)
            op_resid(0, res_next)
            for i in range(NT_):
                xb, xk = xt[i % 2], "xt%d" % (i % 2)
                if i + 1 < NT_:
                    res_next = op_mm(i + 1)
                norm_mod_tile(xb, xk, vecB, vecC, h2f, "h2f", ["vecB", "vecC"])
                if i + 1 < NT_:
                    op_resid(i + 1, res_next)
                router_logits(i, lg_all, h2b_all)
            if b + 1 < NB:
                P.dma("gpsimd", lambda e: e.dma_start(out=winA, in_=win_d[:, 0:1536].rearrange("(k p) n -> p k n", p=128)),
                      "winA", writes=["winA"])
            route_batch(b, lg_all, h2b_all, RS, RB, runall)
            if "x1" in dbg and b == 0:
                P.barrier()
                o = dout("x1", [T, D])
                P.dma("sync", lambda e: e.dma_start(out=o, in_=x1_d[0:T, :]), "dbg_x1", reads=["x1_d"], writes=["dbg_x1"])
            P.barrier(exclude=("scat",))

        if "route" in dbg:
            dbg_store("slots", slots[:], "slots", [128, NTOK // 128, 2], U32)
            dbg_store("wts", wts[:], "wts", [128, NTOK // 128, 2])

        NBLK = ((0, 320), (320, 320))
        if "stop_moe" not in dbg:
            NWB = 3
            NYB = 6
            wgb = [region(0 + 8 * i, [128, 8, DEXP], BF16) for i in range(NWB)]
            wub = [region(24 + 8 * i, [128, 8, DEXP], BF16) for i in range(NWB)]
            wdb = [region(48 + 8 * i, [128, 4, D], BF16) for i in range(NWB)]
            xeb = [region(72, [128, CT, D], BF16), region(82, [128, CT, D], BF16), region(146, [128, CT, D], BF16)]
            xeT = region(92, [128, 8, CAP], BF16)
            hT = region(102, [128, 4, CAP], BF16)
            ysb = [region(108 + 4 * i, [128, D], F32) for i in range(NYB)]
            sgb = [region(132 + 2 * i, [128, 320], F32) for i in range(2)]
            yi = 0
            def moe_wloads(ex):
                w_ = ex % NWB
                P.dma("gpsimd", lambda e: e.dma_start(
                    out=wgb[w_], in_=wg_d[ex * D:(ex + 1) * D, :].rearrange("(k p) n -> p k n", p=128)), "wgb%d" % w_, writes=["wgb%d" % w_])
                P.dma("gpsimd", lambda e: e.dma_start(
                    out=wub[w_], in_=wu_d[ex * D:(ex + 1) * D, :].rearrange("(k p) n -> p k n", p=128)), "wub%d" % w_, writes=["wub%d" % w_])
                P.dma("gpsimd", lambda e: e.dma_start(
                    out=wdb[w_], in_=wd_d[ex * DEXP:(ex + 1) * DEXP, :].rearrange("(k p) n -> p k n", p=128)), "wdb%d" % w_, writes=["wdb%d" % w_])

            def moe_xload(ex):
                s_ = ex % 3
                P.dma("sync", lambda e: e.dma_start(
                    out=xeb[s_], in_=xs_d[ex * CAP:(ex + 1) * CAP, :].rearrange("(t p) d -> p t d", p=128)), "xeb%d" % s_,
                    reads=["xs_d"], writes=["xeb%d" % s_])

            moe_wloads(0)
            moe_wloads(1)
            moe_xload(0)
            moe_xload(1)
            xeTs = [xeT, region(136, [128, 8, CAP], BF16)]

            def moe_tr(ex):
                s_ = ex % 3
                xT, xTk = xeTs[ex % 2], "xeT%d" % (ex % 2)
                for t in range(CT):
                    ps, pk = next_ps()
                    psb = ps[:].bitcast(BF16)
                    for k in range(8):
                        P.op("tensor", lambda e, psb=psb, k=k, t=t: e.transpose(
                            out=psb[:, k * 128:(k + 1) * 128], in_=xeb[s_][:, t, k * 128:(k + 1) * 128], identity=idb[:]),
                            reads=["xeb%d" % s_, "const"] + ([pk] if k else []), writes=[pk])
                    if t % 2 == 0:
                        P.op("scalar", lambda e, psb=psb, t=t: e.activation(
                            out=xT[:, :, t * 128:(t + 1) * 128], in_=psb.rearrange("p (k t) -> p k t", k=8), func=AF.Copy),
                            reads=[pk], writes=[xTk])
                    else:
                        P.op("vector", lambda e, psb=psb, t=t: e.tensor_copy(
                            out=xT[:, :, t * 128:(t + 1) * 128], in_=psb.rearrange("p (k t) -> p k t", k=8)),
                            reads=[pk], writes=[xTk])

            def moe_gu(ex):
                s_ = ex % 2
                w_ = ex % NWB
                xT, xTk = xeTs[s_], "xeT%d" % s_
                for dc in range(4):
                    for bi, (n0, nn) in enumerate(NBLK):
                        psg, kg = next_ps()
                        psu, ku = next_ps()
                        for (ps_, pk_, wb_, wkey) in ((psg, kg, wgb[w_], "wgb%d" % w_), (psu, ku, wub[w_], "wub%d" % w_)):
                            for k in range(8):
                                P.op("tensor", lambda e, ps_=ps_, wb_=wb_, k=k, dc=dc, n0=n0, nn=nn: e.matmul(
                                    ps_[:, 0:nn], lhsT=wb_[:, k, dc * 128:(dc + 1) * 128], rhs=xT[:, k, n0:n0 + nn],
                                    start=(k == 0), stop=(k == 7)), reads=[wkey, xTk] + ([pk_] if k else []), writes=[pk_])
                        sg, sgk = sgb[bi], "sgb%d" % bi
                        P.op("scalar", lambda e, psg=psg, sg=sg, nn=nn: e.activation(out=sg[:, 0:nn], in_=psg[:, 0:nn], func=AF.Silu),
                             reads=[kg], writes=[sgk])
                        P.op("vector", lambda e, psu=psu, sg=sg, dc=dc, n0=n0, nn=nn: e.tensor_tensor(
                            out=hT[:, dc, n0:n0 + nn], in0=sg[:, 0:nn], in1=psu[:, 0:nn], op=ALU.mult),
                            reads=[sgk, ku], writes=["hT"])

            def moe_dn(ex):
                nonlocal_yi = yi_box
                w_ = ex % NWB
                for t in range(CT):
                    yi_ = nonlocal_yi[0]
                    nonlocal_yi[0] += 1
                    yb, yk = ysb[yi_ % NYB], "ysb%d" % (yi_ % NYB)
                    for h in range(2):
                        ps, pk = next_ps()
                        for k in range(4):
                            P.op("tensor", lambda e, ps=ps, k=k, t=t, h=h: e.matmul(
                                ps[:, :], lhsT=hT[:, k, t * 128:(t + 1) * 128], rhs=wdb[w_][:, k, h * 512:(h + 1) * 512],
                                start=(k == 0), stop=(k == 3)), reads=["hT", "wdb%d" % w_] + ([pk] if k else []), writes=[pk])
                        if h == 0:
                            P.op("scalar", lambda e, ps=ps, yb=yb: e.activation(out=yb[:, 0:512], in_=ps[:, :], func=AF.Copy),
                                 reads=[pk], writes=[yk])
                        else:
                            P.op("vector", lambda e, ps=ps, yb=yb: e.tensor_copy(out=yb[:, 512:1024], in_=ps[:, :]),
                                 reads=[pk], writes=[yk])
                    r0 = ex * CAP + t * 128
                    P.dma("sync", lambda e, yb=yb, r0=r0: e.dma_start(out=ys_d[r0:r0 + 128, :], in_=yb[:]), "yst%d" % (yi_ % NYB),
                          reads=[yk], writes=["ys_d"])

            yi_box = [0]
            moe_tr(0)
            for ex in range(NE):
                if ex + 2 < NE:
                    moe_wloads(ex + 2)
                if ex + 2 < NE:
                    moe_xload(ex + 2)
                moe_gu(ex)
                if ex + 1 < NE:
                    moe_tr(ex + 1)
                moe_dn(ex)
            P.barrier()

            NFB = 4
            xfb = [region(0 + 4 * i, [128, D], F32) for i in range(NFB)]
            y0b = [region(16 + 4 * i, [128, D], F32) for i in range(NFB)]
            y1b = [region(32 + 4 * i, [128, D], F32) for i in range(NFB)]
            ob = [region(48 + 4 * i, [128, D], F32) for i in range(2)]
            tB = region(56, [128, D], F32)
            tbufs = [(t1, "t1"), (tB, "tB")]
            for i in range(NFB):
                P.op("gpsimd", lambda e, i=i: e.memset(y0b[i][:], 0.0), writes=["y0b%d" % i])
                P.op("gpsimd", lambda e, i=i: e.memset(y1b[i][:], 0.0), writes=["y1b%d" % i])
            bcast_load(vecB, fg_d[0:1, :], "vecB")
            g2v = [vecA, vecC]
            for b_ in range(NB):
                bcast_load(g2v[b_], mod_d[b_:b_ + 1, 5 * D:6 * D], "vec%s" % "AC"[b_])

            def fin_load(tile):
                j = tile % NFB
                r0 = tile * 128
                P.dma("sync", lambda e: e.dma_start(out=xfb[j][:], in_=x1_d[r0:r0 + 128, :]), "xfb%d" % j,
                      reads=["x1_d"], writes=["xfb%d" % j])
                for (yb_, yk_, kk) in ((y0b[j], "y0b%d" % j, 0), (y1b[j], "y1b%d" % j, 1)):
                    P.dma("gpsimd", lambda e, yb_=yb_, kk=kk: e.indirect_dma_start(
                        out=yb_[:, :], out_offset=None, in_=ys_d[:, :],
                        in_offset=bass.IndirectOffsetOnAxis(ap=slots[:, tile, kk:kk + 1], axis=0),
                        bounds_check=bc_reg(e), oob_is_err=False), yk_, reads=["ys_d", "slots"], writes=[yk_])

            def fin_a0(tile):
                j = tile % NFB
                tt, tk = tbufs[tile % 2]
                P.op("scalar", lambda e: e.activation(out=tt[:], in_=y0b[j][:], func=AF.Copy, scale=wts[:, tile, 0:1]),
                     reads=["y0b%d" % j, "wts"], writes=[tk])

            def fin_a(tile):
                b_ = tile // (T // 128)
                j = tile % NFB
                i2 = tile % 2
                xb, xk = xfb[j], "xfb%d" % j
                y0, y0k, y1, y1k = y0b[j], "y0b%d" % j, y1b[j], "y1b%d" % j
                tt, tk = tbufs[i2]
                gv, gk = g2v[b_], "vec%s" % "AC"[b_]
                P.op("vector", lambda e: e.scalar_tensor_tensor(out=tt[:], in0=y1[:], scalar=wts[:, tile, 1:2], in1=tt[:],
                                                                op0=ALU.mult, op1=ALU.add), reads=[y1k, "wts", tk], writes=[tk])
                P.op("vector", lambda e: e.tensor_tensor(out=tt[:], in0=tt[:], in1=gv[:], op=ALU.mult), reads=[tk, gk], writes=[tk])
                P.op("vector", lambda e: e.tensor_tensor(out=xb[:], in0=xb[:], in1=tt[:], op=ALU.add), reads=[xk, tk], writes=[xk])
                sc = 4 * i2
                P.op("scalar", lambda e: e.activation(out=tt[:], in_=xb[:], func=AF.Square, accum_out=stat[:, sc:sc + 1]),
                     reads=[xk], writes=[tk, "stat%d" % i2])

            def fin_b1(tile):
                i2 = tile % 2
                sk = "stat%d" % i2
                sc = 4 * i2
                P.op("vector", lambda e: e.tensor_scalar(out=stat[:, sc + 1:sc + 2], in0=stat[:, sc:sc + 1], scalar1=1.0 / D, scalar2=EPS,
                                                         op0=ALU.mult, op1=ALU.add), reads=[sk], writes=[sk])
                P.op("scalar", lambda e: e.activation(out=stat[:, sc + 2:sc + 3], in_=stat[:, sc + 1:sc + 2], func=AF.Sqrt),
                     reads=[sk], writes=[sk])

            def fin_b2(tile):
                i2 = tile % 2
                j = tile % NFB
                xb, xk = xfb[j], "xfb%d" % j
                o_, ok_ = ob[i2], "ob%d" % i2
                sk = "stat%d" % i2
                sc = 4 * i2
                r0 = tile * 128
                P.op("vector", lambda e: e.reciprocal(out=stat[:, sc + 3:sc + 4], in_=stat[:, sc + 2:sc + 3]), reads=[sk], writes=[sk])
                P.op("vector", lambda e: e.scalar_tensor_tensor(out=o_[:], in0=xb[:], scalar=stat[:, sc + 3:sc + 4], in1=vecB[:],
                                                                op0=ALU.mult, op1=ALU.mult), reads=[xk, sk, "vecB"], writes=[ok_])
                P.dma("sync", lambda e: e.dma_start(out=out_d[r0:r0 + 128, :], in_=o_[:]), "ost%d" % i2, reads=[ok_], writes=["out_d"])

            NTT = NTOK // 128
            fin_load(0)
            fin_load(1)
            fin_a0(0)
            fin_a(0)
            for tile in range(NTT):
                if tile + 2 < NTT:
                    fin_load(tile + 2)
                if tile + 1 < NTT:
                    fin_a0(tile + 1)
                fin_b1(tile)
                if tile + 1 < NTT:
                    fin_a(tile + 1)
                fin_b2(tile)

        outs = ["dbg_" + k for k in dbg_d] + ["out_d"]
        P.finish(outs)
    return nc, list(dbg_d.keys())


def make_in_maps(inputs):
    f = lambda a: np.ascontiguousarray(np.asarray(a, dtype=np.float32))
    c = _consts()
    x = f(inputs["x"]); cc = f(inputs["c"]); ctx = f(inputs["ctx"]); c_ctx = f(inputs["c_ctx"])
    conv_w = f(inputs["conv_w"])[0]; conv_b = f(inputs["conv_b"])[0]
    ba = f(inputs["lru_ba"])[0]; bx = f(inputs["lru_bx"])[0]; lam = f(inputs["lru_lam"])[0]
    lruc = np.zeros((128, 4, 16), np.float32)
    for ch in range(4):
        sl = slice(ch * 128, (ch + 1) * 128)
        lruc[:, ch, 0:4] = conv_w[:, sl].T
        lruc[:, ch, 4] = conv_b[sl]
        lruc[:, ch, 5] = ba[0, sl]; lruc[:, ch, 6] = ba[1, sl]
        lruc[:, ch, 7] = bx[0, sl]; lruc[:, ch, 8] = bx[1, sl]
        lruc[:, ch, 9] = lam[0, sl]; lruc[:, ch, 10] = lam[1, sl]
    wa = f(inputs["lru_wa"])[0]; wx = f(inputs["lru_wx"])[0]
    wbd = np.zeros((128, 16, 128), np.float32)
    for g, w in enumerate((wa, wx)):
        for d in range(2):
            for ch in range(4):
                idx = (g * 2 + d) * 4 + ch
                for s in range(2):
                    wbd[s * 64:(s + 1) * 64, idx, s * 64:(s + 1) * 64] = w[d, 2 * ch + s]
    wr = np.concatenate([f(inputs["w_group"])[0], f(inputs["w_expert_router"])[0]], axis=1)
    wr = np.ascontiguousarray(wr.reshape(8, 128, 36).transpose(1, 0, 2))
    br = np.concatenate([f(inputs["b_group"])[0], f(inputs["b_expert_router"])[0]])[None, :]
    shared = {
        "w_mod": f(inputs["w_mod"])[0], "b_mod": f(inputs["b_mod"]),
        "norm1_g": f(inputs["norm1_g"]), "norm2_g": f(inputs["norm2_g"]), "final_g": f(inputs["final_g"])[None, :],
        "w_in": f(inputs["w_in"])[0], "w_fo": f(inputs["w_fourier_out"])[0], "w_lo": f(inputs["w_lru_out"])[0],
        "w_out": f(inputs["w_out"])[0], "lruc": lruc, "wbd": wbd, "wr": wr, "br": np.ascontiguousarray(br),
        "w_gate_e": f(inputs["w_gate_e"])[0].reshape(NE * D, DEXP), "w_up_e": f(inputs["w_up_e"])[0].reshape(NE * D, DEXP),
        "w_down_e": f(inputs["w_down_e"])[0].reshape(NE * DEXP, D),
    }
    shared.update(c)
    maps = []
    for core in range(8):
        b0 = core * NB
        cv = np.stack([cc[b0], cc[b0 + 1], c_ctx], axis=1)
        m = dict(shared)
        m["x"] = x[b0:b0 + NB].reshape(NTOK, D)
        m["ctx"] = ctx[b0:b0 + NB].reshape(NB * TC, D)
        m["cT"] = np.ascontiguousarray(cv.reshape(8, 128, 3).transpose(1, 0, 2))
        maps.append(m)
    return maps


def kernel(**inputs):
    nc, _ = build_program()
    maps = make_in_maps(inputs)
    res = run_bass_kernel_spmd(nc, maps, core_ids=list(range(8)))
    out = np.stack([np.asarray(r["out"]).reshape(NB, T, D) for r in res.results], axis=0)
    return out.reshape(16, T, D).astype(np.float32)
```

```python
import contextlib
import numpy as np
import ml_dtypes
import concourse.bass as bass
import concourse.mybir as mybir
from concourse.bass_utils import run_bass_kernel_spmd

F32 = mybir.dt.float32
BF16 = mybir.dt.bfloat16
U32 = mybir.dt.uint32
I32 = mybir.dt.int32
AF = mybir.ActivationFunctionType
ALU = mybir.AluOpType
AX = mybir.AxisListType

ENGS = ("tensor", "vector", "scalar", "gpsimd", "sync")


class Prog:
    def __init__(self, nc, stack):
        self.nc = nc
        self.stack = stack
        self.q = {e: [] for e in ENGS}
        self.esem = {e: stack.enter_context(nc.semaphore("prog_" + e)) for e in ENGS}
        self.ecnt = {e: 0 for e in ENGS}
        self.waited = {e: {} for e in ENGS}
        self.dsem = {}
        self.dcnt = {}
        self.last_w = {}
        self.reads = {}
        self.sems = {}
        for e in ENGS:
            self.sems[id(self.esem[e])] = self.esem[e]

    def _dma_sem(self, key):
        if key not in self.dsem:
            s = self.stack.enter_context(self.nc.semaphore("dma_%d" % len(self.dsem)))
            self.dsem[key] = s
            self.dcnt[key] = 0
            self.sems[id(s)] = s
        return self.dsem[key]

    def _deps(self, eng, reads, writes):
        evs = []
        for r in reads:
            if r in self.last_w:
                evs.append(self.last_w[r])
        for w in writes:
            if w in self.last_w:
                evs.append(self.last_w[w])
            evs.extend(self.reads.get(w, ()))
        need = {}
        for (sid, val) in evs:
            if sid == id(self.esem[eng]) and eng == "tensor":
                continue
            if self.waited[eng].get(sid, 0) >= val:
                continue
            if need.get(sid, 0) < val:
                need[sid] = val
        for sid, val in need.items():
            self.waited[eng][sid] = val
            sem = self.sems[sid]
            self.q[eng].append(lambda e, sem=sem, val=val: e.wait_ge(sem, val))

    def _commit(self, ev, reads, writes):
        for w in writes:
            self.last_w[w] = ev
            self.reads[w] = []
        for r in reads:
            if r in writes:
                continue
            self.reads.setdefault(r, []).append(ev)

    def op(self, eng, fn, reads=(), writes=()):
        reads, writes = tuple(reads), tuple(writes)
        self._deps(eng, reads, writes)
        self.ecnt[eng] += 1
        sem = self.esem[eng]
        self.q[eng].append(lambda e, fn=fn, sem=sem: fn(e).then_inc(sem, 1))
        self._commit((id(sem), self.ecnt[eng]), reads, writes)

    def dma(self, eng, fn, key, reads=(), writes=(), serial=False):
        reads, writes = tuple(reads), tuple(writes)
        sem = self._dma_sem(key)
        prev = self.dcnt[key]
        if serial and prev and self.waited[eng].get(id(sem), 0) < prev:
            self.waited[eng][id(sem)] = prev
            self.q[eng].append(lambda e, sem=sem, val=prev: e.wait_ge(sem, val))
        self._deps(eng, reads, writes)
        self.dcnt[key] += 16
        self.q[eng].append(lambda e, fn=fn, sem=sem: fn(e).then_inc(sem, 16))
        self._commit((id(sem), self.dcnt[key]), reads, writes)

    def finish(self, out_keys):
        self._deps("sync", out_keys, ())
        nc = self.nc
        with nc.Block() as block:
            @block.tensor
            def _(e):
                for f in self.q["tensor"]:
                    f(e)

            @block.vector
            def _(e):
                for f in self.q["vector"]:
                    f(e)

            @block.scalar
            def _(e):
                for f in self.q["scalar"]:
                    f(e)

            @block.gpsimd
            def _(e):
                for f in self.q["gpsimd"]:
                    f(e)

            @block.sync
            def _(e):
                for f in self.q["sync"]:
                    f(e)

    def barrier(self, exclude=()):
        evs = [(id(self.esem[e]), self.ecnt[e]) for e in ENGS if self.ecnt[e]]
        evs += [(id(self.dsem[k]), self.dcnt[k]) for k in self.dsem if self.dcnt[k] and k not in exclude]
        for eng in ENGS:
            for sid, val in evs:
                if sid == id(self.esem[eng]):
                    continue
                if self.waited[eng].get(sid, 0) >= val:
                    continue
                self.waited[eng][sid] = val
                sem = self.sems[sid]
                self.q[eng].append(lambda e, sem=sem, val=val: e.wait_ge(sem, val))


D = 1024
NB = 2
T = 2048
TC = 256
NTOK = NB * T
DIN = 3584
NE = 32
DEXP = 512
CAP = 640
CT = CAP // 128
NSLOT = NE * CAP
EPS = 1e-6
KB = 1024


def _consts():
    c = {}
    ch = np.arange(128)
    th = 2 * np.pi * np.outer(ch, ch) / 128.0
    s = 1.0 / np.sqrt(128.0)
    c["cs"] = np.concatenate([np.cos(th) * s, np.sin(th) * s], axis=1).astype(ml_dtypes.bfloat16)
    p = np.arange(2048)
    r, cc = p // 64, p % 64
    ph = (np.outer(r, r) / 32.0 + np.outer(cc, cc) / 64.0)
    ph = 2 * np.pi * (ph - np.floor(ph))
    s2 = 1.0 / np.sqrt(2048.0)
    c["cp"] = (np.cos(ph) * s2).astype(ml_dtypes.bfloat16)
    c["spn"] = (-np.sin(ph) * s2).astype(ml_dtypes.bfloat16)
    c["idb"] = np.eye(128).astype(ml_dtypes.bfloat16)
    c["idf"] = np.eye(128).astype(np.float32)
    c["ltri"] = np.triu(np.ones((128, 128)), 1).astype(ml_dtypes.bfloat16)
    c["ones"] = np.ones((128, 128)).astype(ml_dtypes.bfloat16)
    c["ebase"] = np.tile((np.arange(NE) * CAP).astype(np.float32)[None, :], (128, 1))
    return c


def build_program(debug=()):
    nc = bass.Bass("TRN2", target_bir_lowering=False)
    dbg = set(debug)

    def din(name, shape, dt=F32):
        return nc.dram_tensor(name, list(shape), dt, kind="ExternalInput").ap()

    x_d = din("x", [NTOK, D])
    ctx_d = din("ctx", [NB * TC, D])
    cT_d = din("cT", [128, 8, 3])
    wmod_d = din("w_mod", [D, 6 * D])
    bmod_d = din("b_mod", [1, 6 * D])
    n1g_d = din("norm1_g", [1, D])
    n2g_d = din("norm2_g", [1, D])
    fg_d = din("final_g", [1, D])
    win_d = din("w_in", [D, DIN])
    wfo_d = din("w_fo", [512, D])
    wlo_d = din("w_lo", [512, D])
    wout_d = din("w_out", [D, D])
    lruc_d = din("lruc", [128, 4, 16])
    wbd_d = din("wbd", [128, 16, 128])
    wr_d = din("wr", [128, 8, 36])
    br_d = din("br", [1, 36])
    wg_d = din("w_gate_e", [NE * D, DEXP])
    wu_d = din("w_up_e", [NE * D, DEXP])
    wd_d = din("w_down_e", [NE * DEXP, D])
    cs_d = din("cs", [128, 256], BF16)
    cp_d = din("cp", [2048, 2048], BF16)
    spn_d = din("spn", [2048, 2048], BF16)
    idb_d = din("idb", [128, 128], BF16)
    idf_d = din("idf", [128, 128], F32)
    ltri_d = din("ltri", [128, 128], BF16)
    ones_d = din("ones", [128, 128], BF16)
    ebase_d = din("ebase", [128, NE], F32)

    out_d = nc.dram_tensor("out", [NTOK, D], F32, kind="ExternalOutput").ap()
    mod_d = nc.dram_tensor("mod_scr", [3, 6 * D], F32, kind="Internal").ap()
    x1_d = nc.dram_tensor("x1_scr", [NTOK, D], F32, kind="Internal").ap()
    xs_d = nc.dram_tensor("xs_scr", [NSLOT, D], BF16, kind="Internal").ap()
    ys_d = nc.dram_tensor("ys_scr", [NSLOT, D], F32, kind="Internal").ap()
    dbg_d = {}

    def dout(name, shape, dt=F32):
        dbg_d[name] = nc.dram_tensor("dbg_" + name, list(shape), dt, kind="ExternalOutput").ap()
        return dbg_d[name]

    with contextlib.ExitStack() as st:
        P = Prog(nc, st)

        def sb(name, shape, dt):
            return st.enter_context(nc.sbuf_tensor("s_" + name, list(shape), dt))

        arena = sb("arena", [128, 160 * KB // 4], F32)

        def region(off_kb, shape, dt):
            n = int(np.prod(shape[1:]))
            if dt == F32:
                v = arena[:, off_kb * 256: off_kb * 256 + n]
            else:
                v = arena[:, off_kb * 256: off_kb * 256 + n // 2].bitcast(BF16)
            if len(shape) == 3:
                v = v.rearrange("p (a b) -> p a b", a=shape[1])
            elif len(shape) == 4:
                v = v.rearrange("p (a b c) -> p a b c", a=shape[1], b=shape[2])
            elif len(shape) == 5:
                v = v.rearrange("p (a b c d) -> p a b c d", a=shape[1], b=shape[2], c=shape[3])
            return v

        psum = [st.enter_context(nc.psum_tensor("ps%d" % i, [128, 512], F32)) for i in range(8)]
        ps_i = [0]

        def next_ps():
            i = ps_i[0]
            ps_i[0] = (i + 1) % 8
            return psum[i], "ps%d" % i

        idb = sb("idb", [128, 128], BF16)
        idf = sb("idf", [128, 128], F32)
        cs = sb("cs", [128, 256], BF16)
        ltri = sb("ltri", [128, 128], BF16)
        ones = sb("ones", [128, 128], BF16)
        ebase = sb("ebase", [128, NE], F32)
        lruc = sb("lruc", [128, 4, 16], F32)
        lrud = sb("lrud", [128, 4, 8], F32)
        wbd = sb("wbd", [128, 16, 128], BF16)
        wr = sb("wr", [128, 8, 36], F32)
        brr = sb("brr", [128, 36], F32)
        onecol = sb("onecol", [128, 1], F32)
        for t_, d_, q in ((idb, idb_d, "sync"), (idf, idf_d, "sync"), (cs, cs_d, "sync"), (ltri, ltri_d, "sync"),
                          (ones, ones_d, "sync"), (ebase, ebase_d, "sync"), (lruc, lruc_d, "sync"),
                          (wr, wr_d, "sync"), (wbd, wbd_d, "gpsimd")):
            P.dma(q, lambda e, t_=t_, d_=d_: e.dma_start(out=t_[:], in_=d_), "const_" + q, writes=["const"])
        P.dma("sync", lambda e: e.dma_start(out=brr[:], in_=br_d.partition_broadcast(128)), "const_sync", writes=["const"])
        P.op("vector", lambda e: e.memset(onecol[:], 1.0), writes=["const"])
        P.op("scalar", lambda e: e.activation(out=lrud[:, :, 0:2], in_=lruc[:, :, 9:11], func=AF.Exp, scale=-1.0),
             reads=["const"], writes=["lrud"])
        P.op("scalar", lambda e: e.activation(out=lrud[:, :, 0:2], in_=lrud[:, :, 0:2], func=AF.Ln, bias=onecol[:, 0:1]),
             reads=["const"], writes=["lrud"])
        P.op("vector", lambda e: e.tensor_scalar(out=lrud[:, :, 2:4], in0=lrud[:, :, 0:2], scalar1=-8.0, scalar2=None, op0=ALU.mult),
             reads=["lrud"], writes=["lrud2"])
        P.op("vector", lambda e: e.tensor_scalar(out=lrud[:, :, 0:2], in0=lrud[:, :, 0:2], scalar1=-4.0, scalar2=None, op0=ALU.mult),
             reads=["lrud", "lrud2"], writes=["lrud"])
        P.op("vector", lambda e: e.tensor_scalar(out=lrud[:, :, 4:8], in0=lruc[:, :, 5:9], scalar1=0.5, scalar2=None, op0=ALU.mult),
             reads=["const", "lrud"], writes=["lrud"])

        def _init_route():
            P.op("vector", lambda e: e.tensor_copy(out=runb[:], in_=ebase[:]), reads=["const"], writes=["runb"])
            P.op("vector", lambda e: e.tensor_scalar(out=ecap[:], in0=ebase[:], scalar1=float(CAP), scalar2=None, op0=ALU.add),
                 reads=["const"], writes=["const"])

        xt = [sb("xt%d" % i, [128, D], F32) for i in range(2)]
        t1 = sb("t1", [128, D], F32)
        hxb = sb("hxb", [128, D], BF16)
        vecA = sb("vecA", [128, D], F32)
        vecB = sb("vecB", [128, D], F32)
        vecC = sb("vecC", [128, D], F32)
        h2T = sb("h2T", [128, 8, 128], F32)
        h2f = sb("h2f", [128, D], F32)
        ecap = sb("ecap", [128, NE], F32)
        stat = sb("stat", [128, 8], F32)
        h0 = sb("h0", [128, 4, 2, NB], F32)
        rt = sb("rt", [128, 256], F32)
        rtb = sb("rtb", [128, 64], BF16)
        runb = sb("runb", [128, NE], F32)
        slots = sb("slots", [128, NTOK // 128, 2], U32)
        wts = sb("wts", [128, NTOK // 128, 2], F32)

        _init_route()

        def bcast_load(dst, src_row, key, q="sync", reads=()):
            P.dma(q, lambda e: e.dma_start(out=dst[:], in_=src_row.partition_broadcast(128)), key, reads=list(reads), writes=[key])

        if True:
            cT = region(0, [128, 8, 3], F32)
            scT = region(1, [128, 8, 3], BF16)
            modsb = region(4, [3, 6 * D], F32)
            bmod3 = region(28, [3, 6 * D], F32)
            wblk = [region(64 + 8 * i, [128, 8, 512], BF16) for i in range(2)]
            P.dma("sync", lambda e: e.dma_start(out=cT, in_=cT_d), "cT", writes=["cT"])
            P.dma("sync", lambda e: e.dma_start(out=bmod3[0:3], in_=bmod_d.partition_broadcast(3)), "bmod3", writes=["bmod3"])
            P.op("scalar", lambda e: e.activation(out=scT, in_=cT, func=AF.Silu), reads=["cT"], writes=["scT"])
            def p0_block(n):
                wb = wblk[n % 2]
                wk = "wblk%d" % (n % 2)
                P.dma("gpsimd", lambda e: e.dma_start(
                    out=wb, in_=wmod_d[:, n * 512:(n + 1) * 512].rearrange("(k p) n -> p k n", p=128)), wk, writes=[wk])
                ps, pk = next_ps()
                for k in range(8):
                    P.op("tensor", lambda e, k=k: e.matmul(ps[0:3, :], lhsT=scT[:, k, :], rhs=wb[:, k, :],
                                                           start=(k == 0), stop=(k == 7)),
                         reads=["scT", wk, pk] if k else ["scT", wk], writes=[pk])
                P.op("vector", lambda e: e.tensor_tensor(out=modsb[0:3, n * 512:(n + 1) * 512], in0=ps[0:3, :],
                                                         in1=bmod3[0:3, n * 512:(n + 1) * 512], op=ALU.add),
                     reads=[pk, "bmod3"], writes=["modsb"])

            for n in range(4):
                p0_block(n)
            P.dma("sync", lambda e: e.dma_start(out=mod_d[:, 0:2 * D], in_=modsb[0:3, 0:2 * D]), "modsbA", reads=["modsb"], writes=["mod_dA"])
            p0_rest = list(range(4, 12))

            def p0_some(k):
                for _ in range(k):
                    if p0_rest:
                        p0_block(p0_rest.pop(0))

        def norm_mod_tile(x, xkey, A, S, dst, dkey, akeys):
            P.op("scalar", lambda e: e.activation(out=t1[:], in_=x[:], func=AF.Square, accum_out=stat[:, 0:1]),
                 reads=[xkey], writes=["t1", "stat"])
            P.op("vector", lambda e: e.tensor_scalar(out=stat[:, 1:2], in0=stat[:, 0:1], scalar1=1.0 / D, scalar2=EPS,
                                                     op0=ALU.mult, op1=ALU.add), reads=["stat"], writes=["stat"])
            P.op("scalar", lambda e: e.activation(out=stat[:, 2:3], in_=stat[:, 1:2], func=AF.Sqrt),
                 reads=["stat"], writes=["stat"])
            P.op("vector", lambda e: e.reciprocal(out=stat[:, 3:4], in_=stat[:, 2:3]), reads=["stat"], writes=["stat"])
            P.op("vector", lambda e: e.scalar_tensor_tensor(out=t1[:], in0=x[:], scalar=stat[:, 3:4], in1=A[:],
                                                            op0=ALU.mult, op1=ALU.mult),
                 reads=[xkey, "stat"] + akeys, writes=["t1"])
            P.op("vector", lambda e: e.tensor_tensor(out=dst[:], in0=t1[:], in1=S[:], op=ALU.add),
                 reads=["t1"] + akeys, writes=[dkey])

        def transpose_bf(src, skey, dstT, dkey):
            ps, pk = next_ps()
            psb = ps[:].bitcast(BF16)
            for k in range(8):
                P.op("tensor", lambda e, k=k: e.transpose(out=psb[:, k * 128:(k + 1) * 128], in_=src[:, k * 128:(k + 1) * 128],
                                                         identity=idb[:]),
                     reads=[skey, "const"] + ([pk] if k else []), writes=[pk])
            P.op("scalar", lambda e: e.activation(out=dstT, in_=psb.rearrange("p (k t) -> p k t", k=8), func=AF.Copy),
                 reads=[pk], writes=[dkey])

        def lru_chunk(u, ukey, Tn, c, init_f, init_b, bufs, okey, nseg=1):
            xc, xcb, rbuf, ig, a, hf, hb = [b[:, 0:Tn] for b in bufs]
            cst = ["const", "lrud", "lrud2"]
            L = Tn // nseg
            P.op("vector", lambda e: e.tensor_scalar(out=xc, in0=u, scalar1=lruc[:, c, 2:3], scalar2=lruc[:, c, 4:5],
                                                     op0=ALU.mult, op1=ALU.add), reads=[ukey] + cst, writes=["xc"])
            xc3 = xc.rearrange("p (s t) -> p s t", s=nseg)
            u3 = u.rearrange("p (s t) -> p s t", s=nseg)
            for (k, o0, o1, i0, i1) in ((0, 2, L, 0, L - 2), (1, 1, L, 0, L - 1), (3, 0, L - 1, 1, L)):
                P.op("vector", lambda e, k=k, o0=o0, o1=o1, i0=i0, i1=i1: e.scalar_tensor_tensor(
                    out=xc3[:, :, o0:o1], in0=u3[:, :, i0:i1], scalar=lruc[:, c, k:k + 1], in1=xc3[:, :, o0:o1],
                    op0=ALU.mult, op1=ALU.add), reads=[ukey, "xc"], writes=["xc"])
            P.op("scalar", lambda e: e.activation(out=xcb, in_=xc, func=AF.Copy), reads=["xc"], writes=["xcb"])
            NH = 2 if (Tn == T and nseg == 1) else 1
            H = Tn // NH
            sets = [(rbuf[:, s0 * H:(s0 + 1) * H], ig[:, s0 * H:(s0 + 1) * H], a[:, s0 * H:(s0 + 1) * H], s0) for s0 in range(NH)] \
                if NH == 2 else [(rbuf, ig, a, 0)]
            if NH == 2:
                sets.append((region(148, [128, H], F32), region(152, [128, H], F32), region(156, [128, H], F32), 2))
            its = []
            for d in range(2):
                order = list(range(NH)) if d == 0 else list(range(NH - 1, -1, -1))
                for oi, hh in enumerate(order):
                    its.append((d, hh, oi, sets[len(its) % len(sets)]))

            def keys(si):
                return "rbuf%d" % si, "ig%d" % si, "abuf%d" % si

            def front(itd):
                d, hh, oi, (rb_, ig_, a_, si) = itd
                rk, ik, ak = keys(si)
                c0 = hh * H
                for tb in range(0, H, 512):
                    n = min(512, H - tb)
                    for (g, dst, dk, bcol) in ((0, rb_, rk, 4 + d), (1, ig_, ik, 6 + d)):
                        ps, pk = next_ps()
                        P.op("tensor", lambda e, ps=ps, g=g, tb=tb, n=n: e.matmul(
                            ps[:, 0:n], lhsT=wbd[:, (g * 2 + d) * 4 + c, :], rhs=xcb[:, c0 + tb:c0 + tb + n], start=True, stop=True),
                            reads=["xcb", "const"], writes=[pk])
                        P.op("scalar", lambda e, ps=ps, dst=dst, tb=tb, n=n, bcol=bcol: e.activation(
                            out=dst[:, tb:tb + n], in_=ps[:, 0:n], func=AF.Tanh, scale=0.5, bias=lrud[:, c, bcol:bcol + 1]),
                            reads=[pk] + cst, writes=[dk])
                P.op("scalar", lambda e: e.activation(out=a_, in_=rb_, func=AF.Exp, scale=lrud[:, c, d:d + 1],
                                                      bias=lrud[:, c, d:d + 1]), reads=[rk] + cst, writes=[ak])

            def mid(itd):
                d, hh, oi, (rb_, ig_, a_, si) = itd
                rk, ik, ak = keys(si)
                P.op("vector", lambda e: e.tensor_tensor(out=rb_, in0=a_, in1=a_, op=ALU.mult), reads=[ak, rk], writes=[rk])
                P.op("vector", lambda e: e.tensor_scalar(out=rb_, in0=rb_, scalar1=1.0, scalar2=-1.0, op0=ALU.min, op1=ALU.mult),
                     reads=[rk], writes=[rk])

            def back(itd):
                d, hh, oi, (rb_, ig_, a_, si) = itd
                rk, ik, ak = keys(si)
                c0 = hh * H
                P.op("scalar", lambda e: e.activation(out=rb_, in_=rb_, func=AF.Sqrt, bias=onecol[:, 0:1]),
                     reads=[rk, "const"], writes=[rk])
                P.op("vector", lambda e: e.scalar_tensor_tensor(out=ig_, in0=ig_, scalar=1.0, in1=rb_, op0=ALU.add, op1=ALU.mult),
                     reads=[rk, ik], writes=[ik])
                P.op("vector", lambda e: e.scalar_tensor_tensor(out=ig_, in0=ig_, scalar=0.5, in1=xc[:, c0:c0 + H],
                                                                op0=ALU.mult, op1=ALU.mult), reads=[ik, "xc"], writes=[ik])
                for sg in range(1, nseg):
                    col = sg * L if d == 0 else sg * L - 1
                    P.op("vector", lambda e, col=col: e.memset(a_[:, col:col + 1], 0.0), reads=[ak], writes=[ak])
                if d == 0:
                    ini = init_f if oi == 0 else hf[:, c0 - 1:c0]
                    P.op("vector", lambda e: e.tensor_tensor_scan(
                        out=hf[:, c0:c0 + H], data0=a_, data1=ig_, initial=ini, op0=ALU.mult, op1=ALU.add),
                        reads=[ak, ik, "h0", okey + "f"], writes=[okey + "f"])
                else:
                    ini = init_b if oi == 0 else hb[:, c0 + H:c0 + H + 1]
                    P.op("vector", lambda e: e.tensor_tensor_scan(
                        out=hb[:, c0:c0 + H][:, ::-1], data0=a_[:, ::-1], data1=ig_[:, ::-1], initial=ini,
                        op0=ALU.mult, op1=ALU.add), reads=[ak, ik, "h0", okey + "b"], writes=[okey + "b"])

            front(its[0])
            for k, itd in enumerate(its):
                mid(itd)
                if k + 1 < len(its) and len(sets) > 1:
                    front(its[k + 1])
                back(itd)
                if k + 1 < len(its) and len(sets) == 1:
                    front(its[k + 1])
            return hf, hb

        BIG = 1.0e6
        _bc = {}

        def bc_reg(e):
            if "r" not in _bc:
                _bc["r"] = e.to_reg(NSLOT - 1)
            return _bc["r"]


        def route_tile(tile):
            V = lambda c0, c1: rt[:, c0:c1]
            dv = lambda fn, r=("rt",), w=("rt",): P.op("vector", fn, reads=list(r), writes=list(w))
            for hh in range(2):
                ps, pk = next_ps()
                for k in range(4):
                    kk = hh * 4 + k
                    P.op("tensor", lambda e, ps=ps, k=k, kk=kk: e.transpose(out=ps[:, k * 128:(k + 1) * 128],
                                                                         in_=h2f[:, kk * 128:(kk + 1) * 128], identity=idf[:]),
                         reads=["h2f", "const"] + ([pk] if k else []), writes=[pk])
                P.op("scalar", lambda e, ps=ps, hh=hh: e.activation(
                    out=h2T[:, hh * 4:(hh + 1) * 4, :], in_=ps[:, :].rearrange("p (k t) -> p k t", k=4), func=AF.Copy),
                    reads=[pk], writes=["h2T"])
            ps, pk = next_ps()
            for k in range(8):
                P.op("tensor", lambda e, ps=ps, k=k: e.matmul(ps[:, 0:36], lhsT=h2T[:, k, :], rhs=wr[:, k, :],
                                                              start=(k == 0), stop=(k == 7)),
                     reads=["h2T", "const"] + ([pk] if k else []), writes=[pk])
            lg = V(0, 36)
            dv(lambda e, ps=ps: e.tensor_tensor(out=lg, in0=ps[:, 0:36], in1=brr[:, :], op=ALU.add), r=[pk, "const", "rt"])
            dv(lambda e: e.tensor_reduce(out=V(36, 37), in_=V(0, 4), axis=AX.X, op=ALU.max))
            dv(lambda e: e.tensor_scalar(out=V(40, 44), in0=V(0, 4), scalar1=V(36, 37), scalar2=None, op0=ALU.is_equal))
            dv(lambda e: e.tensor_scalar(out=V(37, 38), in0=V(36, 37), scalar1=-1.0, scalar2=None, op0=ALU.mult))
            P.op("scalar", lambda e: e.activation(out=V(44, 48), in_=V(0, 4), func=AF.Exp, bias=V(37, 38), accum_out=V(38, 39)),
                 reads=["rt"], writes=["rt"])
            dv(lambda e: e.reciprocal(out=V(39, 40), in_=V(38, 39)))
            dv(lambda e: e.tensor_scalar(out=V(48, 56), in0=V(4, 12), scalar1=V(40, 41), scalar2=None, op0=ALU.mult))
            for g in range(1, 4):
                dv(lambda e, g=g: e.scalar_tensor_tensor(out=V(48, 56), in0=V(4 + 8 * g, 12 + 8 * g), scalar=V(40 + g, 41 + g),
                                                         in1=V(48, 56), op0=ALU.mult, op1=ALU.add))
            dv(lambda e: e.max(out=V(56, 64), in_=V(48, 56)))
            dv(lambda e: e.tensor_scalar(out=V(64, 72), in0=V(48, 56), scalar1=V(56, 57), scalar2=None, op0=ALU.is_equal))
            dv(lambda e: e.tensor_scalar(out=V(72, 80), in0=V(48, 56), scalar1=V(57, 58), scalar2=None, op0=ALU.is_equal))
            dv(lambda e: e.tensor_tensor(out=V(80, 81), in0=V(57, 58), in1=V(56, 57), op=ALU.subtract))
            P.op("scalar", lambda e: e.activation(out=V(81, 82), in_=V(80, 81), func=AF.Exp), reads=["rt"], writes=["rt"])
            dv(lambda e: e.tensor_scalar(out=V(82, 83), in0=V(81, 82), scalar1=1.0, scalar2=None, op0=ALU.add))
            dv(lambda e: e.reciprocal(out=V(83, 84), in_=V(82, 83)))
            dv(lambda e: e.tensor_tensor(out=V(84, 85), in0=V(83, 84), in1=V(39, 40), op=ALU.mult))
            dv(lambda e: e.tensor_tensor(out=V(85, 86), in0=V(84, 85), in1=V(81, 82), op=ALU.mult))
            for g in range(4):
                dv(lambda e, g=g: e.tensor_scalar(out=V(96 + 8 * g, 104 + 8 * g), in0=V(64, 72), scalar1=V(40 + g, 41 + g),
                                                  scalar2=None, op0=ALU.mult))
                dv(lambda e, g=g: e.tensor_scalar(out=V(128 + 8 * g, 136 + 8 * g), in0=V(72, 80), scalar1=V(40 + g, 41 + g),
                                                  scalar2=None, op0=ALU.mult))
            dv(lambda e: e.tensor_tensor(out=rtb[:, 0:32], in0=V(96, 128), in1=V(128, 160), op=ALU.add), w=["rtb"])
            ps2, pk2 = next_ps()
            P.op("tensor", lambda e, ps2=ps2: e.matmul(ps2[:, 0:32], lhsT=ltri[:, :], rhs=rtb[:, 0:32], start=True, stop=True),
                 reads=["rtb", "const"], writes=[pk2])
            P.op("tensor", lambda e, ps2=ps2: e.matmul(ps2[:, 32:64], lhsT=ones[:, :], rhs=rtb[:, 0:32], start=True, stop=True),
                 reads=["rtb", "const", pk2], writes=[pk2])
            dv(lambda e, ps2=ps2: e.tensor_tensor(out=V(160, 192), in0=ps2[:, 0:32], in1=runb[:, :], op=ALU.add),
               r=[pk2, "runb", "rt"])
            dv(lambda e, ps2=ps2: e.tensor_tensor(out=runb[:, :], in0=ps2[:, 32:64], in1=runb[:, :], op=ALU.add),
               r=[pk2, "runb", "rt"], w=["runb"])
            dv(lambda e: e.tensor_tensor(out=V(192, 224), in0=V(160, 192), in1=ecap[:, :], op=ALU.is_lt), r=["rt", "const"])
            for k, (oh0, wcol) in enumerate(((96, 84), (128, 85))):
                dv(lambda e, oh0=oh0: e.tensor_tensor(out=V(oh0, oh0 + 32), in0=V(oh0, oh0 + 32), in1=V(192, 224), op=ALU.mult))
                dv(lambda e, oh0=oh0: e.tensor_tensor(out=V(224, 256), in0=V(oh0, oh0 + 32), in1=V(160, 192), op=ALU.mult))
                dv(lambda e: e.tensor_reduce(out=V(86, 87), in_=V(224, 256), axis=AX.X, op=ALU.add))
                dv(lambda e, oh0=oh0: e.tensor_reduce(out=V(87, 88), in_=V(oh0, oh0 + 32), axis=AX.X, op=ALU.add))
                dv(lambda e: e.scalar_tensor_tensor(out=V(88, 89), in0=V(87, 88), scalar=-BIG, in1=V(86, 87),
                                                    op0=ALU.mult, op1=ALU.add))
                dv(lambda e: e.tensor_scalar(out=V(88, 89), in0=V(88, 89), scalar1=BIG, scalar2=None, op0=ALU.add))
                dv(lambda e, k=k: e.tensor_copy(out=slots[:, tile, k:k + 1], in_=V(88, 89)), w=["slots", "rt"])
                dv(lambda e, k=k, wcol=wcol: e.tensor_tensor(out=wts[:, tile, k:k + 1], in0=V(wcol, wcol + 1), in1=V(87, 88),
                                                             op=ALU.mult), w=["wts", "rt"])
                P.dma("gpsimd", lambda e, k=k: e.indirect_dma_start(
                    out=xs_d[:, :], out_offset=bass.IndirectOffsetOnAxis(ap=slots[:, tile, k:k + 1], axis=0),
                    in_=hxb[:, :], in_offset=None, bounds_check=bc_reg(e), oob_is_err=False),
                    "scat", reads=["slots", "hxb"], writes=["xs_d"])

        def router_logits(i, lg_all, h2b_all):
            P.op("scalar", lambda e: e.activation(out=h2b_all[:, i, :], in_=h2f[:], func=AF.Copy), reads=["h2f"], writes=["h2b_all"])
            for hh in range(2):
                ps, pk = next_ps()
                for k in range(4):
                    kk = hh * 4 + k
                    P.op("tensor", lambda e, ps=ps, k=k, kk=kk: e.transpose(out=ps[:, k * 128:(k + 1) * 128],
                                                                         in_=h2f[:, kk * 128:(kk + 1) * 128], identity=idf[:]),
                         reads=["h2f", "const"] + ([pk] if k else []), writes=[pk])
                P.op("scalar", lambda e, ps=ps, hh=hh: e.activation(
                    out=h2T[:, hh * 4:(hh + 1) * 4, :], in_=ps[:, :].rearrange("p (k t) -> p k t", k=4), func=AF.Copy),
                    reads=[pk], writes=["h2T"])
            ps, pk = next_ps()
            for k in range(8):
                P.op("tensor", lambda e, ps=ps, k=k: e.matmul(ps[:, 0:36], lhsT=h2T[:, k, :], rhs=wr[:, k, :],
                                                              start=(k == 0), stop=(k == 7)),
                     reads=["h2T", "const"] + ([pk] if k else []), writes=[pk])
            P.op("vector", lambda e, ps=ps: e.tensor_tensor(out=lg_all[:, i, :], in0=ps[:, 0:36], in1=brr[:, :], op=ALU.add),
                 reads=[pk, "const"], writes=["lg_all"])

        def route_batch(b, lg_all, h2b_all, RS, RB, runall):
            NT_ = T // 128
            V = lambda c0, c1: RS[:, :, c0:c1]
            bc = lambda ap, w: ap.to_broadcast([128, NT_, w])
            dv = lambda fn, r=("rs",), w=("rs",): P.op("vector", fn, reads=list(r), writes=list(w))
            dv(lambda e: e.tensor_reduce(out=V(36, 37), in_=lg_all[:, :, 0:4], axis=AX.X, op=ALU.max), r=["lg_all", "rs"])
            dv(lambda e: e.tensor_tensor(out=V(40, 44), in0=lg_all[:, :, 0:4], in1=bc(V(36, 37), 4), op=ALU.is_equal), r=["lg_all", "rs"])
            dv(lambda e: e.tensor_tensor(out=V(44, 48), in0=lg_all[:, :, 0:4], in1=bc(V(36, 37), 4), op=ALU.subtract), r=["lg_all", "rs"])
            P.op("scalar", lambda e: e.activation(out=V(44, 48), in_=V(44, 48), func=AF.Exp), reads=["rs"], writes=["rs"])
            dv(lambda e: e.tensor_reduce(out=V(38, 39), in_=V(44, 48), axis=AX.X, op=ALU.add))
            dv(lambda e: e.reciprocal(out=V(39, 40), in_=V(38, 39)))
            t48 = RS[:, :, 96:128].rearrange("p i (g j) -> p i g j", g=4)
            dv(lambda e: e.tensor_tensor(out=t48, in0=V(40, 44).unsqueeze(3).to_broadcast([128, NT_, 4, 8]),
                                         in1=lg_all[:, :, 4:36].rearrange("p i (g j) -> p i g j", g=4), op=ALU.mult), r=["lg_all", "rs"])
            dv(lambda e: e.tensor_reduce(out=V(48, 56), in_=t48.rearrange("p i g j -> p i j g"), axis=AX.X, op=ALU.add))
            dv(lambda e: e.tensor_reduce(out=V(91, 92), in_=V(48, 56), axis=AX.X, op=ALU.max))
            dv(lambda e: e.tensor_tensor(out=V(104, 112), in0=V(48, 56), in1=bc(V(91, 92), 8), op=ALU.is_equal))
            dv(lambda e: e.scalar_tensor_tensor(out=V(96, 104), in0=V(104, 112), scalar=-BIG, in1=V(48, 56), op0=ALU.mult, op1=ALU.add))
            dv(lambda e: e.tensor_reduce(out=V(92, 93), in_=V(96, 104), axis=AX.X, op=ALU.max))
            dv(lambda e: e.tensor_tensor(out=V(112, 120), in0=V(96, 104), in1=bc(V(92, 93), 8), op=ALU.is_equal))
            dv(lambda e: e.tensor_tensor(out=V(93, 94), in0=V(92, 93), in1=V(91, 92), op=ALU.subtract))
            P.op("scalar", lambda e: e.activation(out=V(93, 94), in_=V(93, 94), func=AF.Exp), reads=["rs"], writes=["rs"])
            dv(lambda e: e.tensor_scalar(out=V(94, 95), in0=V(93, 94), scalar1=1.0, scalar2=None, op0=ALU.add))
            dv(lambda e: e.reciprocal(out=V(94, 95), in_=V(94, 95)))
            dv(lambda e: e.tensor_tensor(out=V(95, 96), in0=V(94, 95), in1=V(39, 40), op=ALU.mult))
            dv(lambda e: e.tensor_tensor(out=V(37, 38), in0=V(95, 96), in1=V(93, 94), op=ALU.mult))
            for (o0, mcol) in ((128, 104), (160, 112)):
                dv(lambda e, o0=o0, mcol=mcol: e.tensor_tensor(
                    out=RS[:, :, o0:o0 + 32].rearrange("p i (g j) -> p i g j", g=4),
                    in0=V(40, 44).unsqueeze(3).to_broadcast([128, NT_, 4, 8]),
                    in1=V(mcol, mcol + 8).unsqueeze(2).to_broadcast([128, NT_, 4, 8]), op=ALU.mult))
            dv(lambda e: e.tensor_tensor(out=RB[:, :, :], in0=V(128, 160), in1=V(160, 192), op=ALU.add), w=["rb"])
            psp, kp = next_ps()
            pst, kt = next_ps()
            rbf = RB[:, :, :].rearrange("p i e -> p (i e)")
            P.op("tensor", lambda e: e.matmul(psp[:, :], lhsT=ltri[:, :], rhs=rbf, start=True, stop=True), reads=["rb", "const"], writes=[kp])
            P.op("tensor", lambda e: e.matmul(pst[:, :], lhsT=ones[:, :], rhs=rbf, start=True, stop=True), reads=["rb", "const"], writes=[kt])
            dv(lambda e: e.tensor_copy(out=runall[:, 0, :], in_=runb[:, :]), r=["runb"], w=["runall"])
            for i in range(NT_):
                dv(lambda e, i=i: e.tensor_tensor(out=runall[:, i + 1, :], in0=runall[:, i, :], in1=pst[:, i * 32:(i + 1) * 32], op=ALU.add),
                   r=["runall", kt], w=["runall"])
            dv(lambda e: e.tensor_copy(out=runb[:, :], in_=runall[:, NT_, :]), r=["runall"], w=["runb"])
            dv(lambda e: e.tensor_tensor(out=V(192, 224), in0=psp[:, :].rearrange("p (i e) -> p i e", e=32), in1=runall[:, 0:NT_, :], op=ALU.add),
               r=[kp, "runall", "rs"])
            dv(lambda e: e.tensor_tensor(out=V(224, 256), in0=V(192, 224), in1=ecap[:, :].unsqueeze(1).to_broadcast([128, NT_, 32]), op=ALU.is_lt),
               r=["rs", "const"])
            t0_ = b * NT_
            for k, (o0, wcol) in enumerate(((128, 95), (160, 37))):
                dv(lambda e, o0=o0: e.tensor_tensor(out=V(o0, o0 + 32), in0=V(o0, o0 + 32), in1=V(224, 256), op=ALU.mult))
                dv(lambda e, o0=o0: e.tensor_tensor(out=V(56, 88), in0=V(o0, o0 + 32), in1=V(192, 224), op=ALU.mult))
                dv(lambda e: e.tensor_reduce(out=V(88, 89), in_=V(56, 88), axis=AX.X, op=ALU.add))
                dv(lambda e, o0=o0: e.tensor_reduce(out=V(89, 90), in_=V(o0, o0 + 32), axis=AX.X, op=ALU.add))
                dv(lambda e: e.scalar_tensor_tensor(out=V(90, 91), in0=V(89, 90), scalar=-BIG, in1=V(88, 89), op0=ALU.mult, op1=ALU.add))
                dv(lambda e: e.tensor_scalar(out=V(90, 91), in0=V(90, 91), scalar1=BIG, scalar2=None, op0=ALU.add))
                dv(lambda e, k=k: e.tensor_copy(out=slots[:, t0_:t0_ + NT_, k:k + 1], in_=V(90, 91)), w=["slots", "rs"])
                dv(lambda e, k=k, wcol=wcol: e.tensor_tensor(out=wts[:, t0_:t0_ + NT_, k:k + 1], in0=V(wcol, wcol + 1), in1=V(89, 90), op=ALU.mult),
                   w=["wts", "rs"])
            for i in range(NT_):
                for k in range(2):
                    P.dma("gpsimd", lambda e, i=i, k=k: e.indirect_dma_start(
                        out=xs_d[:, :], out_offset=bass.IndirectOffsetOnAxis(ap=slots[:, t0_ + i, k:k + 1], axis=0),
                        in_=h2b_all[:, i, :], in_offset=None, bounds_check=bc_reg(e), oob_is_err=False),
                        "scat", reads=["slots", "h2b_all"], writes=["xs_d"])

        def lru_bufs(off_kb, Tn=T):
            return [region(off_kb, [128, Tn], F32), region(off_kb + 8, [128, Tn], BF16), region(off_kb + 12, [128, Tn], F32),
                    region(off_kb + 20, [128, Tn], F32), region(off_kb + 28, [128, Tn], F32), region(off_kb + 36, [128, Tn], F32),
                    region(off_kb + 44, [128, Tn], F32)]

        def dbg_store(name, src, skey, shape, dt=F32):
            o = dout(name, shape, dt)
            P.dma("sync", lambda e: e.dma_start(out=o, in_=src), "dbg_" + name, reads=[skey], writes=["dbg_" + name])

        if True:
            bcast_load(vecC, n1g_d[0:1, :], "vecC")
            bcast_load(vecA, mod_d[2:3, D:2 * D], "vecA", reads=["mod_dA"])
            bcast_load(vecB, mod_d[2:3, 0:D], "vecB", reads=["mod_dA"])
            P.op("vector", lambda e: e.scalar_tensor_tensor(out=vecA[:], in0=vecA[:], scalar=1.0, in1=vecC[:],
                                                            op0=ALU.add, op1=ALU.mult), reads=["vecA", "vecC"], writes=["vecA"])
            hcT = region(84, [128, 8, 512], BF16)
            winR = region(92, [128, 8, 512], BF16)
            uct = region(100, [128, 4, 512], F32)
            lb = lru_bufs(108, NB * TC)
            P.dma("gpsimd", lambda e: e.dma_start(out=winR, in_=win_d[:, 512:1024].rearrange("(k p) n -> p k n", p=128)),
                  "winR", writes=["winR"])
            for i in range(4):
                xb = xt[i % 2]
                xk = "xt%d" % (i % 2)
                P.dma("sync", lambda e, xb=xb, i=i: e.dma_start(out=xb[:], in_=ctx_d[i * 128:(i + 1) * 128, :]), xk, writes=[xk])
                norm_mod_tile(xb, xk, vecA, vecB, hxb, "hxb", ["vecA", "vecB"])
                transpose_bf(hxb, "hxb", hcT[:, :, i * 128:(i + 1) * 128], "hcT")
            p0_some(2)
            for c in range(4):
                ps, pk = next_ps()
                for k in range(8):
                    P.op("tensor", lambda e, ps=ps, c=c, k=k: e.matmul(ps[:, :], lhsT=winR[:, k, c * 128:(c + 1) * 128],
                                                                      rhs=hcT[:, k, :], start=(k == 0), stop=(k == 7)),
                         reads=["winR", "hcT"] + ([pk] if k else []), writes=[pk])
                P.op("scalar", lambda e, ps=ps, c=c: e.activation(out=uct[:, c, :], in_=ps[:, :], func=AF.Copy),
                     reads=[pk], writes=["uct"])
            for c in range(4):
                hf, hb = lru_chunk(uct[:, c, :], "uct", NB * TC, c, 0.0, 0.0, lb, "hc", nseg=NB)
                P.op("vector", lambda e, hf=hf, c=c: e.tensor_copy(
                    out=h0[:, c, 0, :], in_=hf.rearrange("p (s t) -> p s t", s=NB)[:, :, TC - 1]), reads=["hcf"], writes=["h0"])
                P.op("vector", lambda e, hb=hb, c=c: e.tensor_copy(
                    out=h0[:, c, 1, :], in_=hb.rearrange("p (s t) -> p s t", s=NB)[:, :, 0]), reads=["hcb"], writes=["h0"])
                p0_some(2)
            p0_some(8)
            P.dma("sync", lambda e: e.dma_start(out=mod_d[:, 2 * D:6 * D], in_=modsb[0:3, 2 * D:6 * D]), "modsbB", reads=["modsb"], writes=["mod_d"])
            if "h0" in dbg:
                dbg_store("h0", h0[:], "h0", [128, 4, 2, NB])
            P.barrier()

        hxT = region(0, [128, 8, T], BF16)
        ufT = region(32, [128, 4, T], BF16)
        gyT = region(48, [128, 4, T], BF16)
        urT = region(64, [128, 4, T], F32)
        winA = region(96, [128, 8, 1536], BF16)
        for b in range(NB if "stop_p1" not in dbg else 0):
            bcast_load(vecC, n1g_d[0:1, :], "vecC")
            bcast_load(vecA, mod_d[b:b + 1, D:2 * D], "vecA")
            bcast_load(vecB, mod_d[b:b + 1, 0:D], "vecB")
            P.op("vector", lambda e: e.scalar_tensor_tensor(out=vecA[:], in0=vecA[:], scalar=1.0, in1=vecC[:],
                                                            op0=ALU.add, op1=ALU.mult), reads=["vecA", "vecC"], writes=["vecA"])
            if b == 0:
                P.dma("gpsimd", lambda e: e.dma_start(out=winA, in_=win_d[:, 0:1536].rearrange("(k p) n -> p k n", p=128)),
                      "winA", writes=["winA"])
            xs1 = [(xt[0], "xt0"), (xt[1], "xt1"), (region(120, [128, D], F32), "xt2")]
            ts1 = [(t1, "t1"), (region(124, [128, D], F32), "t1b")]
            hs1 = [(hxb[:], "hxb"), (h2f[:, 0:D // 2].bitcast(BF16), "h2f")]

            def s1_load(i):
                xb, xk = xs1[i % 3]
                r0 = b * T + i * 128
                P.dma("sync", lambda e: e.dma_start(out=xb[:], in_=x_d[r0:r0 + 128, :]), xk, writes=[xk])

            def s1_stat(i):
                xb, xk = xs1[i % 3]
                tt, tk = ts1[i % 2]
                sc, sk = 4 * (i % 2), "stat%d" % (i % 2)
                P.op("scalar", lambda e: e.activation(out=tt[:], in_=xb[:], func=AF.Square, accum_out=stat[:, sc:sc + 1]),
                     reads=[xk], writes=[tk, sk])
                P.op("vector", lambda e: e.tensor_scalar(out=stat[:, sc + 1:sc + 2], in0=stat[:, sc:sc + 1], scalar1=1.0 / D, scalar2=EPS,
                                                         op0=ALU.mult, op1=ALU.add), reads=[sk], writes=[sk])
                P.op("scalar", lambda e: e.activation(out=stat[:, sc + 2:sc + 3], in_=stat[:, sc + 1:sc + 2], func=AF.Sqrt),
                     reads=[sk], writes=[sk])
                P.op("vector", lambda e: e.reciprocal(out=stat[:, sc + 3:sc + 4], in_=stat[:, sc + 2:sc + 3]), reads=[sk], writes=[sk])

            def s1_apply(i):
                xb, xk = xs1[i % 3]
                tt, tk = ts1[i % 2]
                hb_, hk = hs1[i % 2]
                sc, sk = 4 * (i % 2), "stat%d" % (i % 2)
                P.op("vector", lambda e: e.scalar_tensor_tensor(out=tt[:], in0=xb[:], scalar=stat[:, sc + 3:sc + 4], in1=vecA[:],
                                                                op0=ALU.mult, op1=ALU.mult), reads=[xk, sk, "vecA", tk], writes=[tk])
                P.op("vector", lambda e: e.tensor_tensor(out=hb_, in0=tt[:], in1=vecB[:], op=ALU.add), reads=[tk, "vecB"], writes=[hk])
                transpose_bf(hb_, hk, hxT[:, :, i * 128:(i + 1) * 128], "hxT")

            NT1 = T // 128
            s1_load(0)
            s1_load(1)
            s1_stat(0)
            for i in range(NT1):
                if i + 2 < NT1:
                    s1_load(i + 2)
                if i + 1 < NT1:
                    s1_stat(i + 1)
                s1_apply(i)
            for j in range(12):
                for tb in range(4):
                    ps, pk = next_ps()
                    for k in range(8):
                        P.op("tensor", lambda e, ps=ps, j=j, tb=tb, k=k: e.matmul(
                            ps[:, :], lhsT=winA[:, k, j * 128:(j + 1) * 128], rhs=hxT[:, k, tb * 512:(tb + 1) * 512],
                            start=(k == 0), stop=(k == 7)), reads=["winA", "hxT"] + ([pk] if k else []), writes=[pk])
                    ts = slice(tb * 512, (tb + 1) * 512)
                    if j < 4:
                        P.op("scalar", lambda e, ps=ps, j=j, ts=ts: e.activation(out=ufT[:, j, ts], in_=ps[:, :], func=AF.Copy),
                             reads=[pk], writes=["ufT"])
                    elif j < 8:
                        P.op("vector", lambda e, ps=ps, j=j, ts=ts: e.tensor_copy(out=urT[:, j - 4, ts], in_=ps[:, :]),
                             reads=[pk], writes=["urT"])
                    else:
                        P.op("scalar", lambda e, ps=ps, j=j, ts=ts: e.activation(out=gyT[:, j - 8, ts], in_=ps[:, :], func=AF.Gelu),
                             reads=[pk], writes=["gyT"])
            if "proj" in dbg and b == 0:
                dbg_store("ufT", ufT, "ufT", [128, 4, T], BF16)
                dbg_store("urT", urT, "urT", [128, 4, T])
                dbg_store("gyT", gyT, "gyT", [128, 4, T], BF16)
            P.barrier()
            lb = lru_bufs(96)
            if "lru" in dbg and b == 0:
                o_hf = dout("hf", [128, 4, T])
                o_hb = dout("hb", [128, 4, T])
            for c in range(4):
                hf, hb = lru_chunk(urT[:, c, :], "urT", T, c, h0[:, c, 0, b:b + 1], h0[:, c, 1, b:b + 1], lb, "hl")
                if "lru" in dbg and b == 0:
                    P.dma("sync", lambda e, hf=hf, c=c: e.dma_start(out=o_hf[:, c, :], in_=hf), "dbg_hf", reads=["hlf"], writes=["dbg_hf"])
                    P.dma("sync", lambda e, hb=hb, c=c: e.dma_start(out=o_hb[:, c, :], in_=hb), "dbg_hb", reads=["hlb"], writes=["dbg_hb"])
                P.op("vector", lambda e, hf=hf, hb=hb: e.tensor_tensor(out=hf, in0=hf, in1=hb, op=ALU.add),
                     reads=["hlf", "hlb"], writes=["hlf"])
                P.op("vector", lambda e, hf=hf, c=c: e.tensor_tensor(out=gyT[:, c, :], in0=hf, in1=gyT[:, c, :], op=ALU.mult),
                     reads=["hlf", "gyT"], writes=["gyT"])
            P.barrier()
            if "stop_s4" in dbg:
                break
            Adft = region(64, [128, 16, 2, 4, 128], BF16)
            cpb = [region(96 + 8 * i, [128, 16, 256], BF16) for i in range(2)]
            spb = [region(112 + 8 * i, [128, 16, 256], BF16) for i in range(2)]
            fmT = region(128, [128, 4, T], BF16)
            fmtok = [region(144 + 1 * i, [128, 512], BF16) for i in range(2)]
            ev = 0
            for j in range(16):
                for half in range(2):
                    ps, pk = next_ps()
                    for gg in range(2):
                        g = 2 * half + gg
                        P.op("tensor", lambda e, ps=ps, g=g, gg=gg, j=j: e.matmul(
                            ps[:, gg * 256:(gg + 1) * 256], lhsT=ufT[:, g, j * 128:(j + 1) * 128], rhs=cs[:, :],
                            start=True, stop=True), reads=["ufT", "const"] + ([pk] if gg else []), writes=[pk])
                    dst = Adft[:, j, :, 2 * half:2 * half + 2, :]
                    src = ps[:, :].rearrange("p (g c x) -> p c g x", g=2, c=2)
                    if ev % 2 == 0:
                        P.op("scalar", lambda e, src=src, dst=dst: e.activation(out=dst, in_=src, func=AF.Copy),
                             reads=[pk], writes=["Adft"])
                    else:
                        P.op("vector", lambda e, src=src, dst=dst: e.tensor_copy(out=dst, in_=src), reads=[pk], writes=["Adft"])
                    ev += 1
            for n in range(8):
                cb, sbf = cpb[n % 2], spb[n % 2]
                ck, sk = "cpb%d" % (n % 2), "spb%d" % (n % 2)
                P.dma("sync", lambda e, cb=cb, n=n: e.dma_start(
                    out=cb, in_=cp_d[:, n * 256:(n + 1) * 256].rearrange("(j p) n -> p j n", p=128)), ck, writes=[ck])
                P.dma("sync", lambda e, sbf=sbf, n=n: e.dma_start(
                    out=sbf, in_=spn_d[:, n * 256:(n + 1) * 256].rearrange("(j p) n -> p j n", p=128)), sk, writes=[sk])
                for mm in range(2):
                    m = 2 * n + mm
                    ps, pk = next_ps()
                    for j in range(16):
                        P.op("tensor", lambda e, ps=ps, j=j, cb=cb, mm=mm: e.matmul(
                            ps[:, :], lhsT=cb[:, j, mm * 128:(mm + 1) * 128], rhs=Adft[:, j, 0, :, :].rearrange("p g x -> p (g x)"),
                            start=(j == 0), stop=False), reads=["Adft", ck] + ([pk] if j else []), writes=[pk])
                        P.op("tensor", lambda e, ps=ps, j=j, sbf=sbf, mm=mm: e.matmul(
                            ps[:, :], lhsT=sbf[:, j, mm * 128:(mm + 1) * 128], rhs=Adft[:, j, 1, :, :].rearrange("p g x -> p (g x)"),
                            start=False, stop=(j == 15)), reads=["Adft", sk, pk], writes=[pk])
                    ft, fk = fmtok[m % 2], "fmtok%d" % (m % 2)
                    P.op("scalar", lambda e, ps=ps, ft=ft: e.activation(out=ft[:, :], in_=ps[:, :], func=AF.Copy), reads=[pk], writes=[fk])
                    ps2, pk2 = next_ps()
                    psb2 = ps2[:].bitcast(BF16)
                    for g in range(4):
                        P.op("tensor", lambda e, psb2=psb2, ft=ft, g=g: e.transpose(
                            out=psb2[:, g * 128:(g + 1) * 128], in_=ft[:, g * 128:(g + 1) * 128], identity=idb[:]),
                            reads=[fk, "const"] + ([pk2] if g else []), writes=[pk2])
                    P.op("vector", lambda e, psb2=psb2, m=m: e.tensor_copy(
                        out=fmT[:, :, m * 128:(m + 1) * 128], in_=psb2[:, 0:512].rearrange("p (g t) -> p g t", g=4)),
                        reads=[pk2], writes=["fmT"])
            if "fm" in dbg and b == 0:
                dbg_store("fmT", fmT, "fmT", [128, 4, T], BF16)
            P.barrier()
            mT = region(64, [128, 8, T], BF16)
            winB = region(96, [128, 8, 2048], BF16)
            wfo = region(144, [128, 4, D], BF16)
            wlo = region(152, [128, 4, D], BF16)
            wout = region(32, [128, 8, D], BF16)
            P.dma("gpsimd", lambda e: e.dma_start(out=winB, in_=win_d[:, 1536:3584].rearrange("(k p) n -> p k n", p=128)),
                  "winB", writes=["winB"])
            P.dma("gpsimd", lambda e: e.dma_start(out=wfo, in_=wfo_d.rearrange("(k p) n -> p k n", p=128)), "wfo", writes=["wfo"])
            P.dma("gpsimd", lambda e: e.dma_start(out=wlo, in_=wlo_d.rearrange("(k p) n -> p k n", p=128)), "wlo", writes=["wlo"])
            P.dma("gpsimd", lambda e: e.dma_start(out=wout, in_=wout_d.rearrange("(k p) n -> p k n", p=128)), "wout", writes=["wout"])
            tmps = [(t1, "t1"), (xt[0], "xt0"), (xt[1], "xt1")]
            it = 0
            for dc in range(8):
                dsl = slice(dc * 128, (dc + 1) * 128)
                for tb in range(4):
                    ts = slice(tb * 512, (tb + 1) * 512)
                    tmp, tk = tmps[it % 3]
                    it += 1
                    pss = []
                    for (wt_, wk_, off, src, sk_, nk) in ((wfo, "wfo", 0, fmT, "fmT", 4), (winB, "winB", 0, hxT, "hxT", 8),
                                                          (wlo, "wlo", 0, gyT, "gyT", 4), (winB, "winB", 1024, hxT, "hxT", 8)):
                        ps, pk = next_ps()
                        for k in range(nk):
                            P.op("tensor", lambda e, ps=ps, wt_=wt_, off=off, src=src, k=k, dc=dc, ts=ts, nk=nk: e.matmul(
                                ps[:, :], lhsT=wt_[:, k, off + dc * 128:off + (dc + 1) * 128], rhs=src[:, k, ts],
                                start=(k == 0), stop=(k == nk - 1)), reads=[wk_, sk_] + ([pk] if k else []), writes=[pk])
                        pss.append((ps, pk))
                    (pbf, kbf), (pgf, kgf), (pbr, kbr), (pgr, kgr) = pss
                    P.op("scalar", lambda e, tmp=tmp, pgf=pgf: e.activation(out=tmp[:, 0:512], in_=pgf[:, :], func=AF.Sigmoid),
                         reads=[kgf], writes=[tk])
                    P.op("scalar", lambda e, tmp=tmp, pgr=pgr: e.activation(out=tmp[:, 512:1024], in_=pgr[:, :], func=AF.Sigmoid),
                         reads=[kgr], writes=[tk])
                    P.op("vector", lambda e, tmp=tmp, pbf=pbf: e.tensor_tensor(out=tmp[:, 0:512], in0=tmp[:, 0:512], in1=pbf[:, :],
                                                                             op=ALU.mult), reads=[tk, kbf], writes=[tk])
                    P.op("vector", lambda e, tmp=tmp, pbr=pbr: e.tensor_tensor(out=tmp[:, 512:1024], in0=tmp[:, 512:1024], in1=pbr[:, :],
                                                                             op=ALU.mult), reads=[tk, kbr], writes=[tk])
                    P.op("gpsimd", lambda e, tmp=tmp, dc=dc, ts=ts: e.tensor_tensor(out=mT[:, dc, ts], in0=tmp[:, 0:512],
                                                                                  in1=tmp[:, 512:1024], op=ALU.add),
                         reads=[tk], writes=["mT"])
            P.barrier()
            h2b_all = region(128, [128, T // 128, D], BF16)
            RS = region(0, [128, T // 128, 256], F32)
            RB = region(16, [128, T // 128, 32], BF16)
            lg_all = region(17, [128, T // 128, 36], F32)
            runall = region(20, [128, T // 128 + 1, 32], F32)
            bcast_load(vecA, mod_d[b:b + 1, 2 * D:3 * D], "vecA")
            bcast_load(vecB, mod_d[b:b + 1, 4 * D:5 * D], "vecB")
            bcast_load(vecC, n2g_d[0:1, :], "vecC")
            P.op("vector", lambda e: e.scalar_tensor_tensor(out=vecB[:], in0=vecB[:], scalar=1.0, in1=vecC[:],
                                                            op0=ALU.add, op1=ALU.mult), reads=["vecB", "vecC"], writes=["vecB"])
            bcast_load(vecC, mod_d[b:b + 1, 3 * D:4 * D], "vecC")
            tA = region(48, [128, D], F32)

            def op_mm(i):
                xb, xk = xt[i % 2], "xt%d" % (i % 2)
                r0 = b * T + i * 128
                P.dma("sync", lambda e: e.dma_start(out=xb[:], in_=x_d[r0:r0 + 128, :]), xk, writes=[xk])
                res = []
                for h in range(2):
                    ps, pk = next_ps()
                    for k in range(8):
                        P.op("tensor", lambda e, ps=ps, k=k, h=h: e.matmul(
                            ps[:, :], lhsT=mT[:, k, i * 128:(i + 1) * 128], rhs=wout[:, k, h * 512:(h + 1) * 512],
                            start=(k == 0), stop=(k == 7)), reads=["mT", "wout"] + ([pk] if k else []), writes=[pk])
                    res.append((ps, pk))
                return res

            def op_resid(i, res):
                xb, xk = xt[i % 2], "xt%d" % (i % 2)
                r0 = b * T + i * 128
                for h, (ps, pk) in enumerate(res):
                    P.op("vector", lambda e, ps=ps, h=h: e.tensor_tensor(out=tA[:, h * 512:(h + 1) * 512], in0=ps[:, :],
                                                                       in1=vecA[:, h * 512:(h + 1) * 512], op=ALU.mult),
                         reads=[pk, "vecA"], writes=["tA"])
                P.op("vector", lambda e: e.tensor_tensor(out=xb[:], in0=xb[:], in1=tA[:], op=ALU.add), reads=[xk, "tA"], writes=[xk])
                P.dma("sync", lambda e: e.dma_start(out=x1_d[r0:r0 + 128, :], in_=xb[:]), "x1st%d" % (i % 2),
                      reads=[xk], writes=["x1_d"])

            NT_ = T // 128
            res_next = op_mm(0)
            op_resid(0, res_next)
            for i in range(NT_):
                xb, xk = xt[i % 2], "xt%d" % (i % 2)
                if i + 1 < NT_:
                    res_next = op_mm(i + 1)
                norm_mod_tile(xb, xk, vecB, vecC, h2f, "h2f", ["vecB", "vecC"])
                if i + 1 < NT_:
                    op_resid(i + 1, res_next)
                router_logits(i, lg_all, h2b_all)
            if b + 1 < NB:
                P.dma("gpsimd", lambda e: e.dma_start(out=winA, in_=win_d[:, 0:1536].rearrange("(k p) n -> p k n", p=128)),
                      "winA", writes=["winA"])
            route_batch(b, lg_all, h2b_all, RS, RB, runall)
            if "x1" in dbg and b == 0:
                P.barrier()
                o = dout("x1", [T, D])
                P.dma("sync", lambda e: e.dma_start(out=o, in_=x1_d[0:T, :]), "dbg_x1", reads=["x1_d"], writes=["dbg_x1"])
            P.barrier(exclude=("scat",))

        if "route" in dbg:
            dbg_store("slots", slots[:], "slots", [128, NTOK // 128, 2], U32)
            dbg_store("wts", wts[:], "wts", [128, NTOK // 128, 2])

        NBLK = ((0, 320), (320, 320))
        if "stop_moe" not in dbg:
            NWB = 3
            NYB = 6
            wgb = [region(0 + 8 * i, [128, 8, DEXP], BF16) for i in range(NWB)]
            wub = [region(24 + 8 * i, [128, 8, DEXP], BF16) for i in range(NWB)]
            wdb = [region(48 + 8 * i, [128, 4, D], BF16) for i in range(NWB)]
            xeb = [region(72, [128, CT, D], BF16), region(82, [128, CT, D], BF16), region(146, [128, CT, D], BF16)]
            xeT = region(92, [128, 8, CAP], BF16)
            hT = region(102, [128, 4, CAP], BF16)
            ysb = [region(108 + 4 * i, [128, D], F32) for i in range(NYB)]
            sgb = [region(132 + 2 * i, [128, 320], F32) for i in range(2)]
            yi = 0
            def moe_wloads(ex):
                w_ = ex % NWB
                P.dma("gpsimd", lambda e: e.dma_start(
                    out=wgb[w_], in_=wg_d[ex * D:(ex + 1) * D, :].rearrange("(k p) n -> p k n", p=128)), "wgb%d" % w_, writes=["wgb%d" % w_])
                P.dma("gpsimd", lambda e: e.dma_start(
                    out=wub[w_], in_=wu_d[ex * D:(ex + 1) * D, :].rearrange("(k p) n -> p k n", p=128)), "wub%d" % w_, writes=["wub%d" % w_])
                P.dma("gpsimd", lambda e: e.dma_start(
                    out=wdb[w_], in_=wd_d[ex * DEXP:(ex + 1) * DEXP, :].rearrange("(k p) n -> p k n", p=128)), "wdb%d" % w_, writes=["wdb%d" % w_])

            def moe_xload(ex):
                s_ = ex % 3
                P.dma("sync", lambda e: e.dma_start(
                    out=xeb[s_], in_=xs_d[ex * CAP:(ex + 1) * CAP, :].rearrange("(t p) d -> p t d", p=128)), "xeb%d" % s_,
                    reads=["xs_d"], writes=["xeb%d" % s_])

            moe_wloads(0)
            moe_wloads(1)
            moe_xload(0)
            moe_xload(1)
            xeTs = [xeT, region(136, [128, 8, CAP], BF16)]

            def moe_tr(ex):
                s_ = ex % 3
                xT, xTk = xeTs[ex % 2], "xeT%d" % (ex % 2)
                for t in range(CT):
                    ps, pk = next_ps()
                    psb = ps[:].bitcast(BF16)
                    for k in range(8):
                        P.op("tensor", lambda e, psb=psb, k=k, t=t: e.transpose(
                            out=psb[:, k * 128:(k + 1) * 128], in_=xeb[s_][:, t, k * 128:(k + 1) * 128], identity=idb[:]),
                            reads=["xeb%d" % s_, "const"] + ([pk] if k else []), writes=[pk])
                    if t % 2 == 0:
                        P.op("scalar", lambda e, psb=psb, t=t: e.activation(
                            out=xT[:, :, t * 128:(t + 1) * 128], in_=psb.rearrange("p (k t) -> p k t", k=8), func=AF.Copy),
                            reads=[pk], writes=[xTk])
                    else:
                        P.op("vector", lambda e, psb=psb, t=t: e.tensor_copy(
                            out=xT[:, :, t * 128:(t + 1) * 128], in_=psb.rearrange("p (k t) -> p k t", k=8)),
                            reads=[pk], writes=[xTk])

            def moe_gu(ex):
                s_ = ex % 2
                w_ = ex % NWB
                xT, xTk = xeTs[s_], "xeT%d" % s_
                for dc in range(4):
                    for bi, (n0, nn) in enumerate(NBLK):
                        psg, kg = next_ps()
                        psu, ku = next_ps()
                        for (ps_, pk_, wb_, wkey) in ((psg, kg, wgb[w_], "wgb%d" % w_), (psu, ku, wub[w_], "wub%d" % w_)):
                            for k in range(8):
                                P.op("tensor", lambda e, ps_=ps_, wb_=wb_, k=k, dc=dc, n0=n0, nn=nn: e.matmul(
                                    ps_[:, 0:nn], lhsT=wb_[:, k, dc * 128:(dc + 1) * 128], rhs=xT[:, k, n0:n0 + nn],
                                    start=(k == 0), stop=(k == 7)), reads=[wkey, xTk] + ([pk_] if k else []), writes=[pk_])
                        sg, sgk = sgb[bi], "sgb%d" % bi
                        P.op("scalar", lambda e, psg=psg, sg=sg, nn=nn: e.activation(out=sg[:, 0:nn], in_=psg[:, 0:nn], func=AF.Silu),
                             reads=[kg], writes=[sgk])
                        P.op("vector", lambda e, psu=psu, sg=sg, dc=dc, n0=n0, nn=nn: e.tensor_tensor(
                            out=hT[:, dc, n0:n0 + nn], in0=sg[:, 0:nn], in1=psu[:, 0:nn], op=ALU.mult),
                            reads=[sgk, ku], writes=["hT"])

            def moe_dn(ex):
                nonlocal_yi = yi_box
                w_ = ex % NWB
                for t in range(CT):
                    yi_ = nonlocal_yi[0]
                    nonlocal_yi[0] += 1
                    yb, yk = ysb[yi_ % NYB], "ysb%d" % (yi_ % NYB)
                    for h in range(2):
                        ps, pk = next_ps()
                        for k in range(4):
                            P.op("tensor", lambda e, ps=ps, k=k, t=t, h=h: e.matmul(
                                ps[:, :], lhsT=hT[:, k, t * 128:(t + 1) * 128], rhs=wdb[w_][:, k, h * 512:(h + 1) * 512],
                                start=(k == 0), stop=(k == 3)), reads=["hT", "wdb%d" % w_] + ([pk] if k else []), writes=[pk])
                        if h == 0:
                            P.op("scalar", lambda e, ps=ps, yb=yb: e.activation(out=yb[:, 0:512], in_=ps[:, :], func=AF.Copy),
                                 reads=[pk], writes=[yk])
                        else:
                            P.op("vector", lambda e, ps=ps, yb=yb: e.tensor_copy(out=yb[:, 512:1024], in_=ps[:, :]),
                                 reads=[pk], writes=[yk])
                    r0 = ex * CAP + t * 128
                    P.dma("sync", lambda e, yb=yb, r0=r0: e.dma_start(out=ys_d[r0:r0 + 128, :], in_=yb[:]), "yst%d" % (yi_ % NYB),
                          reads=[yk], writes=["ys_d"])

            yi_box = [0]
            moe_tr(0)
            for ex in range(NE):
                if ex + 2 < NE:
                    moe_wloads(ex + 2)
                if ex + 2 < NE:
                    moe_xload(ex + 2)
                moe_gu(ex)
                if ex + 1 < NE:
                    moe_tr(ex + 1)
                moe_dn(ex)
            P.barrier()

            NFB = 4
            xfb = [region(0 + 4 * i, [128, D], F32) for i in range(NFB)]
            y0b = [region(16 + 4 * i, [128, D], F32) for i in range(NFB)]
            y1b = [region(32 + 4 * i, [128, D], F32) for i in range(NFB)]
            ob = [region(48 + 4 * i, [128, D], F32) for i in range(2)]
            tB = region(56, [128, D], F32)
            tbufs = [(t1, "t1"), (tB, "tB")]
            for i in range(NFB):
                P.op("gpsimd", lambda e, i=i: e.memset(y0b[i][:], 0.0), writes=["y0b%d" % i])
                P.op("gpsimd", lambda e, i=i: e.memset(y1b[i][:], 0.0), writes=["y1b%d" % i])
            bcast_load(vecB, fg_d[0:1, :], "vecB")
            g2v = [vecA, vecC]
            for b_ in range(NB):
                bcast_load(g2v[b_], mod_d[b_:b_ + 1, 5 * D:6 * D], "vec%s" % "AC"[b_])

            def fin_load(tile):
                j = tile % NFB
                r0 = tile * 128
                P.dma("sync", lambda e: e.dma_start(out=xfb[j][:], in_=x1_d[r0:r0 + 128, :]), "xfb%d" % j,
                      reads=["x1_d"], writes=["xfb%d" % j])
                for (yb_, yk_, kk) in ((y0b[j], "y0b%d" % j, 0), (y1b[j], "y1b%d" % j, 1)):
                    P.dma("gpsimd", lambda e, yb_=yb_, kk=kk: e.indirect_dma_start(
                        out=yb_[:, :], out_offset=None, in_=ys_d[:, :],
                        in_offset=bass.IndirectOffsetOnAxis(ap=slots[:, tile, kk:kk + 1], axis=0),
                        bounds_check=bc_reg(e), oob_is_err=False), yk_, reads=["ys_d", "slots"], writes=[yk_])

            def fin_a0(tile):
                j = tile % NFB
                tt, tk = tbufs[tile % 2]
                P.op("scalar", lambda e: e.activation(out=tt[:], in_=y0b[j][:], func=AF.Copy, scale=wts[:, tile, 0:1]),
                     reads=["y0b%d" % j, "wts"], writes=[tk])

            def fin_a(tile):
                b_ = tile // (T // 128)
                j = tile % NFB
                i2 = tile % 2
                xb, xk = xfb[j], "xfb%d" % j
                y0, y0k, y1, y1k = y0b[j], "y0b%d" % j, y1b[j], "y1b%d" % j
                tt, tk = tbufs[i2]
                gv, gk = g2v[b_], "vec%s" % "AC"[b_]
                P.op("vector", lambda e: e.scalar_tensor_tensor(out=tt[:], in0=y1[:], scalar=wts[:, tile, 1:2], in1=tt[:],
                                                                op0=ALU.mult, op1=ALU.add), reads=[y1k, "wts", tk], writes=[tk])
                P.op("vector", lambda e: e.tensor_tensor(out=tt[:], in0=tt[:], in1=gv[:], op=ALU.mult), reads=[tk, gk], writes=[tk])
                P.op("vector", lambda e: e.tensor_tensor(out=xb[:], in0=xb[:], in1=tt[:], op=ALU.add), reads=[xk, tk], writes=[xk])
                sc = 4 * i2
                P.op("scalar", lambda e: e.activation(out=tt[:], in_=xb[:], func=AF.Square, accum_out=stat[:, sc:sc + 1]),
                     reads=[xk], writes=[tk, "stat%d" % i2])

            def fin_b1(tile):
                i2 = tile % 2
                sk = "stat%d" % i2
                sc = 4 * i2
                P.op("vector", lambda e: e.tensor_scalar(out=stat[:, sc + 1:sc + 2], in0=stat[:, sc:sc + 1], scalar1=1.0 / D, scalar2=EPS,
                                                         op0=ALU.mult, op1=ALU.add), reads=[sk], writes=[sk])
                P.op("scalar", lambda e: e.activation(out=stat[:, sc + 2:sc + 3], in_=stat[:, sc + 1:sc + 2], func=AF.Sqrt),
                     reads=[sk], writes=[sk])

            def fin_b2(tile):
                i2 = tile % 2
                j = tile % NFB
                xb, xk = xfb[j], "xfb%d" % j
                o_, ok_ = ob[i2], "ob%d" % i2
                sk = "stat%d" % i2
                sc = 4 * i2
                r0 = tile * 128
                P.op("vector", lambda e: e.reciprocal(out=stat[:, sc + 3:sc + 4], in_=stat[:, sc + 2:sc + 3]), reads=[sk], writes=[sk])
                P.op("vector", lambda e: e.scalar_tensor_tensor(out=o_[:], in0=xb[:], scalar=stat[:, sc + 3:sc + 4], in1=vecB[:],
                                                                op0=ALU.mult, op1=ALU.mult), reads=[xk, sk, "vecB"], writes=[ok_])
                P.dma("sync", lambda e: e.dma_start(out=out_d[r0:r0 + 128, :], in_=o_[:]), "ost%d" % i2, reads=[ok_], writes=["out_d"])

            NTT = NTOK // 128
            fin_load(0)
            fin_load(1)
            fin_a0(0)
            fin_a(0)
            for tile in range(NTT):
                if tile + 2 < NTT:
                    fin_load(tile + 2)
                if tile + 1 < NTT:
                    fin_a0(tile + 1)
                fin_b1(tile)
                if tile + 1 < NTT:
                    fin_a(tile + 1)
                fin_b2(tile)

        outs = ["dbg_" + k for k in dbg_d] + ["out_d"]
        P.finish(outs)
    return nc, list(dbg_d.keys())


def make_in_maps(inputs):
    f = lambda a: np.ascontiguousarray(np.asarray(a, dtype=np.float32))
    c = _consts()
    x = f(inputs["x"]); cc = f(inputs["c"]); ctx = f(inputs["ctx"]); c_ctx = f(inputs["c_ctx"])
    conv_w = f(inputs["conv_w"])[0]; conv_b = f(inputs["conv_b"])[0]
    ba = f(inputs["lru_ba"])[0]; bx = f(inputs["lru_bx"])[0]; lam = f(inputs["lru_lam"])[0]
    lruc = np.zeros((128, 4, 16), np.float32)
    for ch in range(4):
        sl = slice(ch * 128, (ch + 1) * 128)
        lruc[:, ch, 0:4] = conv_w[:, sl].T
        lruc[:, ch, 4] = conv_b[sl]
        lruc[:, ch, 5] = ba[0, sl]; lruc[:, ch, 6] = ba[1, sl]
        lruc[:, ch, 7] = bx[0, sl]; lruc[:, ch, 8] = bx[1, sl]
        lruc[:, ch, 9] = lam[0, sl]; lruc[:, ch, 10] = lam[1, sl]
    wa = f(inputs["lru_wa"])[0]; wx = f(inputs["lru_wx"])[0]
    wbd = np.zeros((128, 16, 128), np.float32)
    for g, w in enumerate((wa, wx)):
        for d in range(2):
            for ch in range(4):
                idx = (g * 2 + d) * 4 + ch
                for s in range(2):
                    wbd[s * 64:(s + 1) * 64, idx, s * 64:(s + 1) * 64] = w[d, 2 * ch + s]
    wr = np.concatenate([f(inputs["w_group"])[0], f(inputs["w_expert_router"])[0]], axis=1)
    wr = np.ascontiguousarray(wr.reshape(8, 128, 36).transpose(1, 0, 2))
    br = np.concatenate([f(inputs["b_group"])[0], f(inputs["b_expert_router"])[0]])[None, :]
    shared = {
        "w_mod": f(inputs["w_mod"])[0], "b_mod": f(inputs["b_mod"]),
        "norm1_g": f(inputs["norm1_g"]), "norm2_g": f(inputs["norm2_g"]), "final_g": f(inputs["final_g"])[None, :],
        "w_in": f(inputs["w_in"])[0], "w_fo": f(inputs["w_fourier_out"])[0], "w_lo": f(inputs["w_lru_out"])[0],
        "w_out": f(inputs["w_out"])[0], "lruc": lruc, "wbd": wbd, "wr": wr, "br": np.ascontiguousarray(br),
        "w_gate_e": f(inputs["w_gate_e"])[0].reshape(NE * D, DEXP), "w_up_e": f(inputs["w_up_e"])[0].reshape(NE * D, DEXP),
        "w_down_e": f(inputs["w_down_e"])[0].reshape(NE * DEXP, D),
    }
    shared.update(c)
    maps = []
    for core in range(8):
        b0 = core * NB
        cv = np.stack([cc[b0], cc[b0 + 1], c_ctx], axis=1)
        m = dict(shared)
        m["x"] = x[b0:b0 + NB].reshape(NTOK, D)
        m["ctx"] = ctx[b0:b0 + NB].reshape(NB * TC, D)
        m["cT"] = np.ascontiguousarray(cv.reshape(8, 128, 3).transpose(1, 0, 2))
        maps.append(m)
    return maps


def kernel(**inputs):
    nc, _ = build_program()
    maps = make_in_maps(inputs)
    res = run_bass_kernel_spmd(nc, maps, core_ids=list(range(8)))
    out = np.stack([np.asarray(r["out"]).reshape(NB, T, D) for r in res.results], axis=0)
    return out.reshape(16, T, D).astype(np.float32)
```
